# Optimizing a Trainium2 kernel written in Bass

```python
import jax, jax.numpy as jnp
from jax import lax
import numpy as np

D_MODEL = 1024
BATCH = 16
SEQ = 2048
DEPTH = 2

HEAD_A = 64
N_HEADS_A = 8
C_A = N_HEADS_A * HEAD_A
R_DECAY = 64
R_ICLR = 64
R_VRES = 32
GN_EPS = HEAD_A * 1e-5
C_B = 512
CONV_W = 3
C_C = 512
POOL_WINDOWS = (2, 4, 8, 16)
N_POOL_GROUPS = 4
G_C = C_C // N_POOL_GROUPS
N_BRANCH = 3
BRANCH_W = 512
NORM_EPS = 1e-6

N_RWKV_IN = 4 * C_A + R_DECAY + R_ICLR
N_CONV_IN = 4 * C_B
N_POOL_IN = 2 * C_C
N_GATE_IN = N_BRANCH * D_MODEL
N_IN = N_RWKV_IN + N_CONV_IN + N_POOL_IN + N_GATE_IN

kernel_name = 'hybrid_rwkv7_shortconv_pool_gated_parallel'


def rms_norm(x, g):
    xf = x.astype(jnp.float32)
    y = xf * lax.rsqrt(jnp.mean(xf * xf, axis=-1, keepdims=True) + NORM_EPS)
    return (y * g.astype(jnp.float32)).astype(x.dtype)


def token_shift(p, mu):
    p_prev = jnp.pad(p, ((0, 0), (1, 0), (0, 0)))[:, :-1]
    return p + (p_prev - p) * mu


def _rwkv7_step(state, inp):
    r_t, w_t, k_t, v_t, a_t, b_t = inp
    sa = jnp.einsum('bhvk,bhk->bhv', state, a_t)
    state = (state * w_t[:, :, None, :] + sa[..., None] * b_t[:, :, None, :]
             + v_t[..., None] * k_t[:, :, None, :])
    y_t = jnp.einsum('bhvk,bhk->bhv', state, r_t)
    return state, y_t


def rwkv7_branch(p_rwkv, w0, w_up, a0, a_up, k_k, k_a, r_k, gn_w, gn_b, v_first, vres):
    bsz, slen, _ = p_rwkv.shape
    dt = p_rwkv.dtype
    f32 = jnp.float32
    r, k, v, z, wd, ad = jnp.split(
        p_rwkv, [C_A, 2 * C_A, 3 * C_A, 4 * C_A, 4 * C_A + R_DECAY], axis=-1)
    w = -jax.nn.softplus(-(w0 + jnp.tanh(wd) @ w_up).astype(f32)) - 0.5
    v_layer = v
    if vres is not None:
        vd, v0, v_up = vres
        v = v + (v_first - v) * jax.nn.sigmoid(v0 + vd @ v_up)
    a = jax.nn.sigmoid(a0 + ad @ a_up)
    heads = lambda t: t.reshape(bsz, slen, N_HEADS_A, HEAD_A).astype(f32)
    kk = heads(k * k_k)
    kk = kk / jnp.maximum(jnp.sqrt(jnp.sum(kk * kk, axis=-1, keepdims=True)), 1e-12)
    k = k * (1 + (a - 1) * k_a)
    rh, kh, vh, ah = heads(r), heads(k), heads(v), heads(a)
    decay = heads(jnp.exp(-jnp.exp(w)))
    seq_major = lambda t: jnp.transpose(t, (1, 0, 2, 3))
    state0 = jnp.zeros((bsz, N_HEADS_A, HEAD_A, HEAD_A), f32)
    _, y = lax.scan(_rwkv7_step, state0,
                    (seq_major(rh), seq_major(decay), seq_major(kh), seq_major(vh),
                     seq_major(-kk), seq_major(kk * ah)))
    y = jnp.transpose(y, (1, 0, 2, 3))
    mean = jnp.mean(y, axis=-1, keepdims=True)
    var = jnp.mean(jnp.square(y - mean), axis=-1, keepdims=True)
    y = ((y - mean) * lax.rsqrt(var + GN_EPS)).reshape(bsz, slen, C_A)
    y = y * gn_w.astype(f32) + gn_b.astype(f32)
    bonus = jnp.sum(rh * kh * r_k.astype(f32), axis=-1, keepdims=True) * vh
    y = y + bonus.reshape(bsz, slen, C_A)
    out = y.astype(dt) * jax.nn.silu(z)
    return out, v_layer


def short_conv_branch(p_conv, conv_w):
    b, c, u, z = jnp.split(p_conv, 4, axis=-1)
    cu = c * u
    y = lax.conv_general_dilated(
        cu, conv_w[:, None, :], window_strides=(1,), padding=[(CONV_W - 1, 0)],
        dimension_numbers=('NWC', 'WIO', 'NWC'), feature_group_count=C_B)
    return b * y * jax.nn.silu(z)


def pool_branch(p_pool, pool_w, pool_scale):
    u, z = jnp.split(p_pool, 2, axis=-1)
    bsz, slen, _ = u.shape
    f32 = jnp.float32
    ug = u.reshape(bsz, slen, N_POOL_GROUPS, G_C).astype(f32)
    csum = jnp.cumsum(ug, axis=1)
    t = jnp.arange(1, slen + 1, dtype=f32)
    outs = []
    for gi, win in enumerate(POOL_WINDOWS):
        cg = csum[:, :, gi]
        prev = jnp.pad(cg, ((0, 0), (win, 0), (0, 0)))[:, :slen]
        cnt = jnp.minimum(t, float(win))[None, :, None]
        outs.append((cg - prev) / cnt - ug[:, :, gi])
    pooled = jnp.stack(outs, axis=2)
    mixed = jnp.einsum('bsgc,gcd->bsgd', pooled, pool_w.astype(f32)).reshape(bsz, slen, C_C)
    return (mixed * pool_scale.astype(f32)).astype(u.dtype) * jax.nn.silu(z)


def setup_inputs(seed: int = 0) -> dict:
    key = jax.random.key(seed)
    ks = iter(jax.random.split(key, 32))
    nrm = lambda shape, scale: scale * jax.random.normal(next(ks), shape, jnp.float32)
    uni = lambda shape, lo, hi: jax.random.uniform(next(ks), shape, jnp.float32, lo, hi)
    L = DEPTH
    Lv = DEPTH - 1
    return {
        'x': nrm((BATCH, SEQ, D_MODEL), 1.0),
        'pre_norm_w': 1.0 + nrm((L, D_MODEL), 0.1),
        'post_norm_w': 1.0 + nrm((L, D_MODEL), 0.1),
        'w_in': nrm((L, D_MODEL, N_IN), D_MODEL ** -0.5),
        'mu_shift': uni((L, N_RWKV_IN), 0.0, 1.0),
        'rwkv_w0': uni((L, C_A), -6.0, -1.0),
        'rwkv_w_up': nrm((L, R_DECAY, C_A), 0.1 * R_DECAY ** -0.5),
        'rwkv_a0': nrm((L, C_A), 0.1),
        'rwkv_a_up': nrm((L, R_ICLR, C_A), R_ICLR ** -0.5),
        'rwkv_k_k': 0.85 + nrm((L, C_A), 0.05),
        'rwkv_k_a': 1.0 + nrm((L, C_A), 0.05),
        'rwkv_r_k': nrm((L, N_HEADS_A, HEAD_A), 0.1),
        'rwkv_gn_w': 1.0 + nrm((L, C_A), 0.1),
        'rwkv_gn_b': nrm((L, C_A), 0.02),
        'vres_down': nrm((Lv, D_MODEL, R_VRES), D_MODEL ** -0.5),
        'vres_mu': uni((Lv, R_VRES), 0.0, 1.0),
        'vres_v0': nrm((Lv, C_A), 0.1),
        'vres_up': nrm((Lv, R_VRES, C_A), R_VRES ** -0.5),
        'conv_w': nrm((L, CONV_W, C_B), CONV_W ** -0.5),
        'pool_w': nrm((L, N_POOL_GROUPS, G_C, G_C), G_C ** -0.5),
        'pool_scale': 1.0 + nrm((L, C_C), 0.1),
        'w_branch': nrm((L, N_BRANCH, BRANCH_W, D_MODEL), BRANCH_W ** -0.5),
        'w_out': nrm((L, D_MODEL, D_MODEL), D_MODEL ** -0.5),
    }


def reference(x, pre_norm_w, post_norm_w, w_in, mu_shift, rwkv_w0, rwkv_w_up, rwkv_a0,
              rwkv_a_up, rwkv_k_k, rwkv_k_a, rwkv_r_k, rwkv_gn_w, rwkv_gn_b, vres_down,
              vres_mu, vres_v0, vres_up, conv_w, pool_w, pool_scale, w_branch, w_out):
    bsz, slen, _ = x.shape
    v_first = None
    for l in range(DEPTH):
        h = rms_norm(x, pre_norm_w[l])
        if l == 0:
            w_cat = w_in[l]
        else:
            w_cat = jnp.concatenate([w_in[l], vres_down[l - 1]], axis=1)
        p = h @ w_cat
        p_rwkv, p_conv, p_pool, p_gate = jnp.split(
            p[..., :N_IN],
            [N_RWKV_IN, N_RWKV_IN + N_CONV_IN, N_RWKV_IN + N_CONV_IN + N_POOL_IN], axis=-1)
        p_rwkv = token_shift(p_rwkv, mu_shift[l])
        if l == 0:
            vres = None
        else:
            vres = (token_shift(p[..., N_IN:], vres_mu[l - 1]), vres_v0[l - 1], vres_up[l - 1])
        y_a, v_layer = rwkv7_branch(p_rwkv, rwkv_w0[l], rwkv_w_up[l], rwkv_a0[l], rwkv_a_up[l],
                                    rwkv_k_k[l], rwkv_k_a[l], rwkv_r_k[l], rwkv_gn_w[l],
                                    rwkv_gn_b[l], v_first, vres)
        if l == 0:
            v_first = v_layer
        y_b = short_conv_branch(p_conv, conv_w[l])
        y_c = pool_branch(p_pool, pool_w[l], pool_scale[l])
        ys = jnp.stack([y_a, y_b, y_c], axis=0)
        proj = jnp.einsum('nbsc,ncd->nbsd', ys, w_branch[l])
        gates = jax.nn.sigmoid(p_gate.reshape(bsz, slen, N_BRANCH, D_MODEL))
        merged = jnp.einsum('bsnd,nbsd->bsd', gates, proj)
        out = merged @ w_out[l]
        x = x + rms_norm(out, post_norm_w[l])
    return x
```

```python
import itertools
import math
from contextlib import ExitStack

import numpy as np
import concourse.bass as bass
import concourse.mybir as mybir
from concourse.bass_utils import run_bass_kernel_spmd

F32 = mybir.dt.float32
BF16 = mybir.dt.bfloat16
ALU = mybir.AluOpType
AF = mybir.ActivationFunctionType
AX = mybir.AxisListType

PE, ACT, DVE, POOL, SP = "pe", "act", "dve", "pool", "sp"
COMPUTE = (PE, ACT, DVE, POOL)

D_MODEL = 1024
SEQ = 2048
N_IN = 8320
HALF = 1024
NCL = 66
C0 = -math.exp(-0.5)
GN_EPS = 64 * 1e-5
NORM_EPS = 1e-6
POOL_WINDOWS = (2, 4, 8, 16)
import os as _os
ESHIFT = int(_os.environ.get("KB_ESHIFT", "1"))
EADD = POOL
ABSPLIT = 0


def K(name, *idx):
    dims = []
    for i in idx:
        if isinstance(i, (list, tuple, range)):
            dims.append(list(i))
        else:
            dims.append([i])
    return [(name,) + t for t in itertools.product(*dims)]


class Op:
    __slots__ = ("eng", "fn", "deps", "sig", "idx", "is_dma", "dkey", "dval", "waits")


class Prog:
    def __init__(self, batch_keys=()):
        self.ops = []
        self.last_w = {}
        self.readers = {}
        self.batch_keys = set(batch_keys)
        self._cap = None

    def begin_capture(self):
        self._cap = []

    def end_capture(self):
        c, self._cap = self._cap, None
        return c

    def replay(self, item):
        return self.add(*item)

    def add(self, eng, fn, reads=(), writes=(), is_dma=False, dkey=None):
        if self._cap is not None:
            self._cap.append((eng, fn, list(reads), list(writes), is_dma, dkey))
            return None
        op = Op()
        op.eng, op.fn, op.is_dma, op.dkey = eng, fn, is_dma, dkey
        op.sig, op.dval, op.waits = False, 0, []
        op.idx = len(self.ops)
        if eng != PE:
            pbk = [k for k in reads if k[0] == "pb"]
            if pbk:
                reads = [k for k in reads if k[0] != "pb"]
                writes = list(writes) + pbk
        deps = set()
        for k in reads:
            w = self.last_w.get(k)
            if w is not None:
                deps.add(w)
        for k in writes:
            w = self.last_w.get(k)
            if w is not None:
                deps.add(w)
            for r in self.readers.get(k, ()):
                deps.add(r)
        deps.discard(op.idx)
        op.deps = sorted(deps)
        for k in writes:
            self.last_w[k] = op.idx
            self.readers[k] = []
        for k in reads:
            self.readers.setdefault(k, []).append(op.idx)
        self.ops.append(op)
        return op

    def dma(self, fn, reads=(), writes=(), dkey="dma", eng=SP):
        return self.add(eng, fn, reads, writes, is_dma=True, dkey=dkey)

    def emit(self, nc, sems, dma_sems):
        ops = self.ops
        dcnt = {}
        for op in ops:
            if op.is_dma:
                dcnt[op.dkey] = dcnt.get(op.dkey, 0) + 16
                op.dval = dcnt[op.dkey]
        need = [False] * len(ops)
        waited = {}
        wl = [None] * len(ops)
        for op in ops:
            w = {}
            for d in op.deps:
                dop = ops[d]
                if dop.is_dma:
                    key = ("d", dop.dkey)
                    r = dcnt[dop.dkey] if dop.dkey in self.batch_keys else dop.dval
                else:
                    if dop.eng == PE and op.eng == PE and not op.is_dma:
                        continue
                    key = ("c", dop.eng)
                    r = d
                if r > w.get(key, -1):
                    w[key] = r
            ew = waited.setdefault(op.eng, {})
            lst = []
            for key, r in w.items():
                if ew.get(key, -1) >= r:
                    continue
                ew[key] = r
                lst.append((key, r))
                if key[0] == "c":
                    need[r] = True
            wl[op.idx] = lst
        cnt = {e: 0 for e in COMPUTE}
        val = [0] * len(ops)
        for op in ops:
            if not op.is_dma and need[op.idx]:
                cnt[op.eng] += 1
                val[op.idx] = cnt[op.eng]
                op.sig = True
        for op in ops:
            op.waits = [(key, (val[r] if key[0] == "c" else r)) for key, r in wl[op.idx]]
        self.final_cnt = (dict(cnt), dict(dcnt))
        by_eng = {}
        for op in ops:
            by_eng.setdefault(op.eng, []).append(op)

        def run(engname, e):
            for op in by_eng.get(engname, []):
                for key, v in op.waits:
                    s = dma_sems[key[1]] if key[0] == "d" else sems[key[1]]
                    e.wait_ge(s, v)
                ins = op.fn(e)
                if op.is_dma:
                    ins.then_inc(dma_sems[op.dkey], 16)
                elif op.sig:
                    ins.then_inc(sems[op.eng], 1)

        with nc.Block() as block:
            @block.tensor
            def _(e):
                run(PE, e)

            @block.scalar
            def _(e):
                run(ACT, e)

            @block.vector
            def _(e):
                run(DVE, e)

            @block.gpsimd
            def _(e):
                run(POOL, e)

            @block.sync
            def _(e):
                run(SP, e)
                for k, v in dcnt.items():
                    e.wait_ge(dma_sems[k], v)
        return {e: len(v) for e, v in by_eng.items()}


def build_program(n_seq=2, n_half=2, n_layers=2, dbg=False):
    nc = bass.Bass("TRN2", target_bir_lowering=False)
    NTOK = n_seq * SEQ

    def din(name, shape):
        return nc.dram_tensor(name, shape, F32, kind="ExternalInput").ap()

    x_d = din("x", [NTOK, D_MODEL])
    w_in_d = din("w_in", [2, D_MODEL, N_IN])
    w_br_d = din("w_branch", [2, 3, 512, D_MODEL])
    w_out_d = din("w_out", [2, D_MODEL, D_MODEL])
    vdn_d = din("vres_down", [D_MODEL, 32])
    lw_d = din("lw", [2, 128, 512])
    vup_d = din("vres_up", [32, 512])
    pw_d = din("pool_w", [2, 4, 128, 128])
    cols_d = din("cols", [128, 2 * NCL])
    gpre_d = din("gpre", [2, D_MODEL])
    gpost_d = din("gpost", [2, D_MODEL])
    identf_d = din("identf", [128, 128])
    mask4_d = din("mask4", [128, 512])
    maskl_d = din("maskl", [128, 128])
    bones_d = din("bones", [128, 128])
    rmask_d = din("rmask", [128, 512])
    icnt_d = din("icnt", [128, 64])
    y_d = nc.dram_tensor("y", [NTOK, D_MODEL], F32, kind="ExternalOutput").ap()
    if dbg:
        dbg_d = nc.dram_tensor("dbg", [3, 128, 4, HALF], F32, kind="ExternalOutput").ap()

    NST, NWB = 3, 8
    dkeys = ["const", "xin", "xout", "gpre", "gpost", "dbg"] + ["st%d" % i for i in range(NST)]
    P = Prog(batch_keys=["const"])

    with ExitStack() as es:
        def sb(name, shape, dt=F32):
            return es.enter_context(nc.sbuf_tensor(name, shape, dt))

        XS = sb("XS", [128, 8, D_MODEL])
        HT = sb("HT", [128, 8, HALF], BF16)
        YA = sb("YA", [128, 4, HALF], BF16)
        YB = sb("YB", [128, 4, HALF], BF16)
        YC = sb("YC", [128, 4, HALF], BF16)
        VF = sb("VF", [128, 4, HALF], BF16)
        ARENA = sb("ARENA", [128, 17 * 512])
        G = [ARENA[:, g * 512:(g + 1) * 512] for g in range(17)]
        MG = ARENA[:, 0:4096].bitcast(BF16).rearrange("p (k t) -> p k t", k=8)
        WO = ARENA[:, 4096:8192].bitcast(BF16).rearrange("p (k t) -> p k t", k=8)
        BQ = [sb("BQ%d" % i, [128, 528]) for i in range(4)]
        ART = sb("ART", [128, 4, 2, 128], BF16)
        BKT = sb("BKT", [128, 4, 2, 128], BF16)
        BKTOK = sb("BKTOK", [128, 4, 2, 128], BF16)
        VTOK = sb("VTOK", [128, 4, 128], BF16)
        AB = [sb("AB%d" % u, [128, 512], BF16) for u in range(8)]
        NN = [sb("NN%d" % u, [128, 128], BF16) for u in range(8)]
        PTP = [sb("PTP%d" % p_, [128, 256], BF16) for p_ in range(4)]
        PT = [PTP[u // 2][:, (u % 2) * 128:(u % 2 + 1) * 128] for u in range(8)]
        MMP = [sb("MMP%d" % p_, [128, 512], BF16) for p_ in range(4)]
        XSB = sb("XSB", [128, 2, 64], BF16)
        USB = sb("USB", [128, 2, 64], BF16)
        STB = [sb("STB%d" % i, [128, 64], BF16) for i in range(2)]
        STG = sb("STG", [128, 64])
        S32 = [sb("S32_%d" % l, [128, 4, 64]) for l in range(2)]
        YN = sb("YN", [128, 4, 128])
        BONX = sb("BONX", [128, 512])
        ZGX = sb("ZGX", [128, 512])
        JNK = sb("JNK", [128, 64])
        GC = sb("GC", [128, 4])
        SUM8 = sb("SUM8", [128, 8])
        SSQ8 = sb("SSQ8", [128, 8])
        M8 = sb("M8", [128, 8])
        V8 = sb("V8", [128, 8])
        SS1 = sb("SS1", [128, 2])
        RST = sb("RST", [128, 2])
        SS2 = sb("SS2", [128, 2])
        RS2 = sb("RS2", [128, 1])
        CAR = [sb("CAR%d" % l, [128, 18]) for l in range(2)]
        CCAR = [sb("CCAR%d" % l, [128, 4, 2]) for l in range(2)]
        PCAR = [sb("PCAR%d" % l, [128, 4, 15]) for l in range(2)]
        LD = sb("LD", [128, HALF], BF16)
        PLBALL = sb("PLBALL", [128, 2, 512], BF16)
        PLB = [PLBALL[:, i, :] for i in range(2)]
        VD = PLBALL[0:32].rearrange("p n t -> p (n t)")
        WBR2 = [sb("WBR2_%d" % i, [128, 4, 128], BF16) for i in range(2)]
        IDF = sb("IDF", [128, 128])
        IDB = sb("IDB", [128, 128], BF16)
        MASK4 = sb("MASK4", [128, 512], BF16)
        MASKL = sb("MASKL", [128, 128], BF16)
        BONES = sb("BONES", [128, 128])
        RMASK = sb("RMASK", [128, 512], BF16)
        ICNT = sb("ICNT", [128, 4, 16])
        COLS = sb("COLS", [128, 2 * NCL])
        DER = sb("DER", [128, 2 * NCL])
        LW = [sb("LW%d" % l, [128, 512], BF16) for l in range(2)]
        VU = sb("VU", [32, 512], BF16)
        PW = [sb("PW%d" % l, [128, 4, 128], BF16) for l in range(2)]
        GPRE = sb("GPRE", [128, D_MODEL])
        GPOST = sb("GPOST", [128, D_MODEL])
        STAGE = [sb("STAGE%d" % i, [128, 8, 128]) for i in range(NST)]
        WBF = [sb("WBF%d" % i, [128, 8, 128], BF16) for i in range(NWB)]
        pb = [es.enter_context(nc.psum_tensor("pb%d" % i, [128, 512], F32)) for i in range(8)]

        sems = {e: es.enter_context(nc.semaphore("s_" + e)) for e in COMPUTE}
        dsem = {k: es.enter_context(nc.semaphore("d_" + k)) for k in dkeys}

        def act(out, in_, func, reads, writes, **kw):
            P.add(ACT, lambda e: e.activation(out=out, in_=in_, func=func, **kw), reads, writes)

        def tt(eng, out, in0, in1, op, reads, writes):
            P.add(eng, lambda e: e.tensor_tensor(out=out, in0=in0, in1=in1, op=op), reads, writes)

        def ts(eng, out, in0, s1, s2, op0, op1, reads, writes):
            if s2 is None:
                P.add(eng, lambda e: e.tensor_scalar(out=out, in0=in0, scalar1=s1, scalar2=None, op0=op0), reads, writes)
            else:
                P.add(eng, lambda e: e.tensor_scalar(out=out, in0=in0, scalar1=s1, scalar2=s2, op0=op0, op1=op1), reads, writes)

        def stt(out, in0, scalar, in1, op0, op1, reads, writes):
            P.add(DVE, lambda e: e.scalar_tensor_tensor(out=out, in0=in0, scalar=scalar, in1=in1, op0=op0, op1=op1),
                  reads, writes)

        def cp(eng, out, in_, reads, writes):
            if eng == ACT:
                act(out, in_, AF.Copy, reads, writes)
            else:
                P.add(eng, lambda e: e.tensor_copy(out=out, in_=in_), reads, writes)

        def mm(out, lhsT, rhs, start, stop, reads, writes):
            P.add(PE, lambda e: e.matmul(out, lhsT=lhsT, rhs=rhs, start=start, stop=stop), reads, writes)

        def tr(out, in_, ident, reads, writes):
            P.add(PE, lambda e: e.transpose(out=out, in_=in_, identity=ident), reads, writes)

        def col(l, c):
            return COLS[:, l * NCL + c: l * NCL + c + 1]

        def dcol(l, c):
            return DER[:, l * NCL + c: l * NCL + c + 1]

        KG = lambda *g: K("G", list(g))
        Kpb = lambda *b: K("pb", list(b))

        wstate = {"st": 0, "wb": 0}

        def wload(src, nk=8, ncol=128, dst=None, dst_keys=None, k0=0, same_stage=False):
            if same_stage:
                s = (wstate["st"] - 1) % NST
            else:
                s = wstate["st"] % NST
                wstate["st"] += 1
            P.dma(lambda e: e.dma_start(out=STAGE[s][:, k0:k0 + nk, 0:ncol], in_=src),
                  writes=K("st", s, list(range(k0, k0 + nk))), dkey="st%d" % s)
            return s

        def wcast(s, nk=8, ncol=128, dst=None, dst_keys=None, eng=POOL):
            if dst is None:
                w = wstate["wb"] % NWB
                wstate["wb"] += 1
                dst = WBF[w][:, 0:nk, 0:ncol]
                dst_keys = K("wb", w)
            else:
                w = None
            src_ = STAGE[s][:, 0:nk, 0:ncol]
            if ncol == 128 and w is not None:
                dst = WBF[w][:, 0:nk, :].rearrange("p k n -> p (k n)")
                src_ = STAGE[s][:, 0:nk, :].rearrange("p k n -> p (k n)")
            cp(eng, dst, src_, K("st", s, list(range(nk))), dst_keys)
            return w

        def w_in_chunk(l, c0, ncol=128):
            return w_in_d[l].rearrange("(k p) n -> p k n", p=128)[:, :, c0:c0 + ncol]

        def load_in_chunks(l, c0s):
            ws = []
            for c0 in c0s:
                s = wload(w_in_chunk(l, c0))
                ws.append(wcast(s))
            return ws

        def cdma(out, in_, wkeys):
            P.dma(lambda e: e.dma_start(out=out, in_=in_), writes=wkeys, dkey="const")

        cdma(IDF[:], identf_d, K("IDF"))
        cdma(G[5][:, 0:512], mask4_d, KG(5))
        cdma(G[6][:, 0:128], maskl_d, KG(6))
        cdma(BONES[:], bones_d, K("BONES"))
        cdma(G[7][:, 0:512], rmask_d, KG(7))
        cdma(ICNT[:].rearrange("p g t -> p (g t)"), icnt_d, K("ICNT"))
        cdma(COLS[:], cols_d, K("COLS"))
        cdma(G[0][:, 0:512], lw_d[0], KG(0))
        cdma(G[1][:, 0:512], lw_d[1], KG(1))
        cdma(G[2][0:32, 0:512], vup_d, KG(2))
        cdma(G[3].rearrange("p (g d) -> p g d", g=4), pw_d[0].rearrange("g c d -> c g d"), KG(3))
        cdma(G[4].rearrange("p (g d) -> p g d", g=4), pw_d[1].rearrange("g c d -> c g d"), KG(4))
        cp(POOL, LW[0][:], G[0], KG(0), K("LW", 0))
        cp(POOL, LW[1][:], G[1], KG(1), K("LW", 1))
        cp(POOL, VU[:], G[2][0:32, :], KG(2), K("VU"))
        cp(POOL, PW[0][:].rearrange("p g d -> p (g d)"), G[3], KG(3), K("PW", 0))
        cp(POOL, PW[1][:].rearrange("p g d -> p (g d)"), G[4], KG(4), K("PW", 1))
        cp(POOL, IDB[:], IDF[:], K("IDF"), K("IDB"))
        cp(POOL, MASK4[:], G[5], KG(5), K("MASK4"))
        cp(POOL, MASKL[:], G[6][:, 0:128], KG(6), K("MASKL"))
        cp(POOL, RMASK[:], G[7], KG(7), K("RMASK"))
        ts(DVE, DER[:], COLS[:], -1.0, 1.0, ALU.mult, ALU.add, K("COLS"), K("DER"))

        def phase_A(l):
            for t in range(8):
                p = t % 2
                sq = ARENA[:, 0:1024]
                xsc = ARENA[:, 1024 + p * 1024: 2048 + p * 1024]
                kx = KG(2 + 2 * p, 3 + 2 * p)
                act(sq, XS[:, t, :], AF.Square, K("XS", t), KG(0, 1) + K("SS1", p), accum_out=SS1[:, p:p + 1])
                act(RST[:, p:p + 1], SS1[:, p:p + 1], AF.Ln, K("SS1", p), K("RST", p), scale=1.0 / D_MODEL, bias=NORM_EPS)
                act(RST[:, p:p + 1], RST[:, p:p + 1], AF.Exp, K("RST", p), K("RST", p), scale=-0.5)
                stt(xsc, XS[:, t, :], RST[:, p:p + 1], GPRE[:], ALU.mult, ALU.mult,
                    K("XS", t) + K("RST", p) + K("GPRE"), kx)
                b0 = 2 * p
                for kc in range(8):
                    tr(pb[b0 + kc // 4][:, (kc % 4) * 128:(kc % 4 + 1) * 128], xsc[:, kc * 128:(kc + 1) * 128], IDF[:],
                       kx + K("IDF"), Kpb(b0 + kc // 4))
                cp(ACT, HT[:, 0:4, t * 128:(t + 1) * 128], pb[b0][:].rearrange("p (k t) -> p k t", k=4), Kpb(b0), K("HT", t))
                cp(DVE, HT[:, 4:8, t * 128:(t + 1) * 128], pb[b0 + 1][:].rearrange("p (k t) -> p k t", k=4), Kpb(b0 + 1), K("HT", t))

        def inproj(bank, w, n, M=128):
            for kc in range(8):
                mm(pb[bank][0:M, :], WBF[w][:, kc, 0:M], HT[:, kc, n * 512:(n + 1) * 512], kc == 0, kc == 7,
                   K("wb", w) + K("HT", list(range(4 * n, 4 * n + 4))), Kpb(bank))

        def shift(l, ci, bank, bq, dst, dkeys, M=128):
            cp(ACT, BQ[bq][0:M, 0:1], CAR[l][0:M, ci:ci + 1], K("CAR", l, ci), K("BQ", bq))
            act(BQ[bq][0:M, 1:513], pb[bank][0:M, :], AF.Copy, Kpb(bank) + K("COLS"), K("BQ", bq), scale=col(l, ci)[0:M])
            cp(ACT, CAR[l][0:M, ci:ci + 1], BQ[bq][0:M, 512:513], K("BQ", bq), K("CAR", l, ci))
            stt(dst, pb[bank][0:M, :], dcol(l, ci)[0:M], BQ[bq][0:M, 0:512], ALU.mult, ALU.add,
                Kpb(bank) + K("DER") + K("BQ", bq), dkeys)

        def phase_B0(l, ws):
            for n in range(2):
                nt = slice(n * 512, (n + 1) * 512)
                inproj(n, ws[0], n)
                shift(l, 16, n, 0, G[0], KG(0))
                act(LD[0:64, nt], G[0][0:64, :], AF.Tanh, KG(0), K("LD", n, 0))
                cp(ACT, LD[64:128, nt], G[0][64:128, :], KG(0), K("LD", n, 1))
                if l == 1:
                    inproj(2 + n, ws[1], n, M=32)
                    shift(l, 17, 2 + n, 1, G[1][0:32, :], KG(1), M=32)
                    cp(ACT, VD[0:32, nt], G[1][0:32, :], KG(1), K("PLB", n))

        def inproj_ops(bank, w, n):
            ops_ = []
            for kc in range(8):
                ops_.append(lambda bank=bank, w=w, n=n, kc=kc: mm(
                    pb[bank][:, :], WBF[w][:, kc, :], HT[:, kc, n * 512:(n + 1) * 512], kc == 0, kc == 7,
                    K("wb", w) + K("HT", list(range(4 * n, 4 * n + 4))), Kpb(bank)))
            return ops_

        bstate = {"pre": None, "shifted": None, "gn": None}

        def phase_B(l, j, ws, first_half, ws_next=None):
            jc = slice(j * 128, (j + 1) * 128)
            R32, K32, V32, Z32, SG, A32, KK, KP, CS, EP, EN, BON, T0, T1, T2, T3, T4 = G
            pb5b = pb[5][:].bitcast(BF16)
            for n in range(2):
                nt = slice(n * 512, (n + 1) * 512)
                if bstate["pre"] != (l, j, n):
                    for q in range(4):
                        inproj(q, ws[q], n)
                bstate["pre"] = None
                if n == 0:
                    nxt_w, nxt_key = ws, (l, j, 1)
                    nxt_n = 1
                elif ws_next is not None:
                    nxt_w, nxt_key = ws_next, (l, j + 1, 0)
                    nxt_n = 0
                else:
                    nxt_w, nxt_key = None, None
                fill = []
                if nxt_w is not None:
                    for q in range(4):
                        fill.extend(inproj_ops(q, nxt_w[q], nxt_n))

                def emit_fill(k):
                    for _ in range(k):
                        if fill:
                            fill.pop(0)()
                mm(pb[4][:], LW[l][0:64, jc], LD[0:64, nt], True, True, K("LW", l) + K("LD", n, 0), Kpb(4))
                mm(pb[5][:], LW[l][64:128, jc], LD[64:128, nt], True, True, K("LW", l) + K("LD", n, 1), Kpb(5))
                if l == 1:
                    mm(pb[6][:], VU[0:32, jc], VD[0:32, nt], True, True, K("VU") + K("PLB", n), Kpb(6))
                if bstate["shifted"] != (l, j, n):
                    for q in range(4):
                        shift(l, q * 4 + j, q, q, G[q], KG(q))
                bstate["shifted"] = None
                BONp, kbon = (BON, KG(11)) if n == 0 else (BONX[:], K("BONX"))
                ZGp, kzg = (T4, KG(16)) if n == 0 else (ZGX[:], K("ZGX"))
                P.begin_capture()
                act(SG, pb[4][:], AF.Sigmoid, Kpb(4) + K("COLS"), KG(4), bias=col(l, 18 + j))
                act(A32, pb[5][:], AF.Sigmoid, Kpb(5) + K("COLS"), KG(5), bias=col(l, 22 + j))
                act(ZGp, Z32, AF.Sigmoid, KG(3), kzg)
                tt(POOL, ZGp, ZGp, Z32, ALU.mult, kzg + KG(3), kzg)
                if l == 0:
                    cp(POOL, VF[:, j, nt], V32, KG(2), K("VF", j, n))
                else:
                    act(T0, pb[6][:], AF.Sigmoid, Kpb(6) + K("COLS"), KG(12), bias=col(l, 46 + j))
                    tt(DVE, T1, VF[:, j, nt], V32, ALU.subtract, K("VF", j, n) + KG(2), KG(13))
                    tt(DVE, T1, T1, T0, ALU.mult, KG(13, 12), KG(13))
                    tt(DVE, V32, V32, T1, ALU.add, KG(2, 13), KG(2))
                act(T2, K32, AF.Square, KG(1) + K("COLS"), KG(14), scale=col(l, 26 + j))
                mm(pb[0][:], BONES[:], T2, True, True, K("BONES") + KG(14), Kpb(0))
                act(T2, pb[0][:], AF.Ln, Kpb(0), KG(14), bias=1e-24)
                act(T2, T2, AF.Exp, KG(14), KG(14), scale=-0.5)
                stt(KK, K32, col(l, 26 + j), T2, ALU.mult, ALU.mult, KG(1, 14) + K("COLS"), KG(6))
                act(T3, A32, AF.Identity, KG(5) + K("COLS") + K("DER"), KG(15), scale=col(l, 30 + j), bias=dcol(l, 30 + j))
                tt(DVE, KP, K32, T3, ALU.mult, KG(1, 15), KG(7))
                tt(DVE, T3, KK, A32, ALU.mult, KG(6, 5, 7), KG(15))
                P.add(DVE, lambda e: e.tensor_tensor_scan(out=CS, data0=RMASK[:], data1=SG, initial=0.0,
                                                          op0=ALU.mult, op1=ALU.add), K("RMASK") + KG(4), KG(8))
                act(EP, CS, AF.Exp, KG(8), KG(9), scale=C0)
                act(EN, CS, AF.Exp, KG(8), KG(10), scale=-C0)
                tt(DVE, T1, CS, SG, ALU.subtract, KG(8, 4), KG(13))
                act(T1, T1, AF.Exp, KG(13), KG(13), scale=C0)
                cp(ACT, GC[:], EP[:, 127::128], KG(9), K("GC"))
                v4 = lambda a: a.rearrange("p (c t) -> p c t", c=4)
                tt(POOL, ART[:, :, 1, :], v4(R32), v4(EP), ALU.mult, KG(0, 9), K("ART", 1))
                stt(ART[:, :, 0, :], v4(KK), -1.0, v4(T1), ALU.mult, ALU.mult, KG(6, 13), K("ART", 0))
                tt(POOL, BKT[:, :, 0, :], v4(T3), v4(EN), ALU.mult, KG(15, 10), K("BKT", 0))
                tt(DVE, BKT[:, :, 1, :], v4(KP), v4(EN), ALU.mult, KG(7, 10), K("BKT", 1))
                ts(POOL, T2, R32, col(l, 34 + j), 1.0, ALU.mult, ALU.mult, KG(0) + K("COLS"), KG(14))
                tt(POOL, T2, T2, KP, ALU.mult, KG(14, 7), KG(14))
                mm(pb[1][:], BONES[:], T2, True, True, K("BONES") + KG(14), Kpb(1))
                tt(DVE, BONp, pb[1][:], V32, ALU.mult, Kpb(1) + KG(2), kbon)
                for c in range(4):
                    for i in range(2):
                        tr(pb5b[:, (c * 2 + i) * 128:(c * 2 + i + 1) * 128], BKT[:, c, i, :], IDB[:],
                           K("BKT", i) + K("IDB"), Kpb(5))
                cp(ACT, BKTOK[:].rearrange("p c i k -> p (c i k)"), pb5b, Kpb(5), K("BKTOK"))
                for c in range(4):
                    tr(pb[6][:, c * 128:(c + 1) * 128], V32[:, c * 128:(c + 1) * 128], IDF[:], KG(2) + K("IDF"), Kpb(6))
                cp(DVE, VTOK[:].rearrange("p c v -> p (c v)"), pb[6][:], Kpb(6), K("VTOK"))
                pre_ops = P.end_capture()
                gn_ops = bstate["gn"] or []
                bstate["gn"] = None
                for i_, it_ in enumerate(pre_ops):
                    P.replay(it_)
                    if gn_ops:
                        P.replay(gn_ops.pop(0))
                for it_ in gn_ops:
                    P.replay(it_)
                units = [(c, h) for c in range(4) for h in range(2)]
                for u, (c, h) in enumerate(units):
                    po = 64 * h
                    ar = ART[po:po + 64, c].rearrange("p a t -> p (a t)")
                    mm(pb[u][:, 0:256], BKT[po:po + 64, c, 0, :], ar, True, True, K("BKT", 0) + K("ART", [0, 1]), Kpb(u))
                    mm(pb[u][:, 256:512], BKT[po:po + 64, c, 1, :], ar, True, True, K("BKT", 1) + K("ART", [0, 1]), Kpb(u))
                for u in range(8):
                    if ABSPLIT:
                        tt(DVE, AB[u][:, 0:256], pb[u][:, 0:256], MASK4[:, 0:256], ALU.mult, Kpb(u) + K("MASK4"), K("AB", u, 0))
                        cp(ACT, AB[u][:, 256:512], pb[u][:, 256:512], Kpb(u), K("AB", u, 1))
                    else:
                        tt(DVE, AB[u][:], pb[u][:], MASK4[:], ALU.mult, Kpb(u) + K("MASK4"), K("AB", u, [0, 1]))
                for u in range(8):
                    if ABSPLIT:
                        tt(POOL, AB[u][:, 256:512], AB[u][:, 256:512], MASK4[:, 256:512], ALU.mult,
                           K("AB", u, 1) + K("MASK4"), K("AB", u, 1))
                for u in range(8):
                    tr(pb[u][:].bitcast(BF16)[:, 0:128], AB[u][:, 0:128], IDB[:], K("AB", u, 0) + K("IDB"), Kpb(u))
                for u in range(8):
                    cp(ACT, NN[u][:], pb[u][:].bitcast(BF16)[:, 0:128], Kpb(u), K("NN", u))
                    tt(DVE, PT[u], AB[u][:, 0:128], IDB[:], ALU.add, K("AB", u, 0) + K("IDB"), K("PTP", u // 2))
                for b in range(1, 8):
                    for p_ in range(4):
                        for i in range(2):
                            u = 2 * p_ + i
                            o = 256 * i
                            if b == 1:
                                Mn, Mt, kin = NN[u][:], AB[u][:, 0:128], K("NN", u) + K("AB", u, 0)
                            else:
                                Mn, Mt, kin = MMP[p_][:, o:o + 128], MMP[p_][:, o + 128:o + 256], K("MMP", p_)
                            if b <= 6:
                                mm(pb[p_][:, o:o + 128], Mt, Mn, True, True, kin, Kpb(p_))
                            if b <= 5:
                                mm(pb[p_][:, o + 128:o + 256], Mn, Mt, True, True, kin, Kpb(p_))
                            if b >= 2:
                                mm(pb[4 + p_][:, i * 128:(i + 1) * 128], Mn, PT[u], True, True, kin + K("PTP", p_), Kpb(4 + p_))
                    for p_ in range(4):
                        ev_ = DVE if p_ == 3 else ACT
                        if b <= 5:
                            cp(ev_, MMP[p_][:], pb[p_][:], Kpb(p_), K("MMP", p_))
                        elif b == 6:
                            cp(ev_, MMP[p_][:].rearrange("p (i x) -> p i x", i=2)[:, :, 0:128],
                               pb[p_][:].rearrange("p (i x) -> p i x", i=2)[:, :, 0:128], Kpb(p_), K("MMP", p_))
                        if b >= 2:
                            tt(DVE, PTP[p_][:], pb[4 + p_][:, 0:256], PTP[p_][:], ALU.add, Kpb(4 + p_) + K("PTP", p_), K("PTP", p_))
                cp(ACT, STB[0][:], S32[l][:, j, :], K("S32", l, j), K("STB", 0))
                for c in range(4):
                    sbi = c % 2
                    act(STG[:], S32[l][:, j, :], AF.Copy, K("S32", l, j) + K("GC"), K("STG"), scale=GC[:, c:c + 1])
                    for h in range(2):
                        u, hs = c * 2 + h, slice(64 * h, 64 * h + 64)
                        xo = pb[4][:, h * 64:(h + 1) * 64]
                        mm(xo, AB[u][:, 256:384], VTOK[:, c, hs], True, False, K("AB", u, 1) + K("VTOK"), Kpb(4))
                        mm(xo, ART[hs, c, 0, :], STB[sbi][hs, :], False, True, K("ART", 0) + K("STB", sbi), Kpb(4))
                    emit_fill(3)
                    cp(ACT, XSB[:].rearrange("p h v -> p (h v)"), pb[4][:, 0:128], Kpb(4), K("XSB"))
                    for h in range(2):
                        u = c * 2 + h
                        mm(pb[5][:, h * 64:(h + 1) * 64], PT[u], XSB[:, h, :], True, True, K("PTP", u // 2) + K("XSB"), Kpb(5))
                    emit_fill(3)
                    cp(DVE, USB[:].rearrange("p h v -> p (h v)"), pb[5][:, 0:128], Kpb(5), K("USB"))
                    for h in range(2):
                        u, hs = c * 2 + h, slice(64 * h, 64 * h + 64)
                        so = pb[6][hs, 0:64]
                        mm(so, BKTOK[:, c, 0, hs], USB[:, h, :], True, False, K("BKTOK") + K("USB"), Kpb(6))
                        mm(so, BKTOK[:, c, 1, hs], VTOK[:, c, hs], False, True, K("BKTOK") + K("VTOK"), Kpb(6))
                    for h in range(2):
                        u, hs = c * 2 + h, slice(64 * h, 64 * h + 64)
                        yo = pb[7][:, u * 64:(u + 1) * 64]
                        mm(yo, ART[hs, c, 1, :], STB[sbi][hs, :], True, False, K("ART", 1) + K("STB", sbi), Kpb(7))
                        mm(yo, AB[u][:, 128:256], USB[:, h, :], False, False, K("AB", u, 0) + K("USB"), Kpb(7))
                        mm(yo, AB[u][:, 384:512], VTOK[:, c, hs], False, True, K("AB", u, 1) + K("VTOK"), Kpb(7))
                    emit_fill(2)
                    stt(S32[l][:, j, :], pb[6][:, 0:64], GC[:, c:c + 1], STG[:], ALU.mult, ALU.add,
                        Kpb(6) + K("GC") + K("STG"), K("S32", l, j))
                    cp(ACT, STB[1 - sbi][:], S32[l][:, j, :], K("S32", l, j), K("STB", 1 - sbi))
                emit_fill(len(fill))
                if nxt_key is not None:
                    bstate["pre"] = nxt_key
                if nxt_key is not None and ESHIFT:
                    for q in range(4):
                        shift(l, q * 4 + nxt_key[1], q, q, G[q], KG(q))
                    bstate["shifted"] = nxt_key
                P.begin_capture()
                y3 = pb[7][:].rearrange("p (g v) -> p g v", g=8)
                P.add(DVE, lambda e: e.tensor_reduce(out=SUM8[:], in_=y3, axis=AX.X, op=ALU.add), Kpb(7), K("SUM8"))
                for u in range(8):
                    act(JNK[:], pb[7][:, u * 64:(u + 1) * 64], AF.Square, Kpb(7), K("JNK") + K("SSQ8"),
                        accum_out=SSQ8[:, u:u + 1])
                ts(DVE, M8[:], SUM8[:], 1.0 / 64, None, ALU.mult, None, K("SUM8"), K("M8"))
                tt(DVE, V8[:], M8[:], M8[:], ALU.mult, K("M8"), K("V8"))
                stt(V8[:], SSQ8[:], 1.0 / 64, V8[:], ALU.mult, ALU.subtract, K("SSQ8") + K("V8"), K("V8"))
                act(V8[:], V8[:], AF.Ln, K("V8"), K("V8"), bias=GN_EPS)
                act(V8[:], V8[:], AF.Exp, K("V8"), K("V8"), scale=-0.5)
                for u, (c, h) in enumerate(units):
                    ts(DVE, YN[:, c, h * 64:(h + 1) * 64], pb[7][:, u * 64:(u + 1) * 64], M8[:, u:u + 1], V8[:, u:u + 1],
                       ALU.subtract, ALU.mult, Kpb(7) + K("M8") + K("V8"), K("YN"))
                for c in range(4):
                    tr(pb[3][:, c * 128:(c + 1) * 128], YN[:, c, :], IDF[:], K("YN") + K("IDF"), Kpb(3))
                YNf = YN[:].rearrange("p c v -> p (c v)")
                stt(YNf, pb[3][:], col(l, 38 + j), BONp, ALU.mult, ALU.add, Kpb(3) + K("COLS") + kbon, K("YN"))
                stt(YA[:, j, nt], YNf, col(l, 42 + j), ZGp, ALU.add, ALU.mult, K("YN") + kzg + K("COLS"), K("YA", j, n))
                bstate["gn"] = P.end_capture()
                if ws_next is None and n == 1:
                    for it_ in bstate["gn"]:
                        P.replay(it_)
                    bstate["gn"] = None

        def phase_C(l, j, ws):
            for n in range(2):
                nt = slice(n * 512, (n + 1) * 512)
                bb, gb = 4 * n, 4 * n
                for q in range(4):
                    inproj(bb + q, ws[q], n)
                U32, Y, SZ = G[gb], G[gb + 1], G[gb + 2]
                cp(ACT, U32, pb[bb + 2][:], Kpb(bb + 2), KG(gb))
                cp(ACT, BQ[n][:, 0:2], CCAR[l][:, j, :], K("CCAR", l, j), K("BQ", n))
                tt(DVE, BQ[n][:, 2:514], pb[bb + 1][:], U32, ALU.mult, Kpb(bb + 1) + KG(gb), K("BQ", n))
                cp(ACT, CCAR[l][:, j, :], BQ[n][:, 512:514], K("BQ", n), K("CCAR", l, j))
                ts(DVE, Y, BQ[n][:, 0:512], col(l, 50 + j), None, ALU.mult, None, K("BQ", n) + K("COLS"), KG(gb + 1))
                stt(Y, BQ[n][:, 1:513], col(l, 54 + j), Y, ALU.mult, ALU.add, K("BQ", n) + K("COLS") + KG(gb + 1), KG(gb + 1))
                stt(Y, BQ[n][:, 2:514], col(l, 58 + j), Y, ALU.mult, ALU.add, K("BQ", n) + K("COLS") + KG(gb + 1), KG(gb + 1))
                act(SZ, pb[bb + 3][:], AF.Silu, Kpb(bb + 3), KG(gb + 2))
                tt(DVE, Y, pb[bb][:], Y, ALU.mult, Kpb(bb) + KG(gb + 1), KG(gb + 1))
                tt(DVE, YB[:, j, nt], Y, SZ, ALU.mult, KG(gb + 1, gb + 2), K("YB", j, n))

        def phase_D(l, g, ws, first_half):
            W = POOL_WINDOWS[g]
            for n in range(2):
                nt = slice(n * 512, (n + 1) * 512)
                bb, gb = 4 * n, 4 * n
                inproj(bb, ws[0], n)
                inproj(bb + 1, ws[1], n)
                cp(ACT, BQ[n][:, 0:15], PCAR[l][:, g, :], K("PCAR", l, g), K("BQ", n))
                cp(ACT, BQ[n][:, 15:527], pb[bb][:], Kpb(bb), K("BQ", n))
                cp(ACT, PCAR[l][:, g, :], BQ[n][:, 512:527], K("BQ", n), K("PCAR", l, g))
                cur, curk, lo = BQ[n], K("BQ", n), 0
                for k in (1, 2, 4, 8):
                    if k >= W:
                        break
                    ni = 2 if curk != K("BQ", 2) else 3
                    nxt, nxtk = BQ[ni], K("BQ", ni)
                    tt(DVE, nxt[:, lo + k:527], cur[:, lo + k:527], cur[:, lo:527 - k], ALU.add, curk, nxtk)
                    cur, curk, lo = nxt, nxtk, lo + k
                stt(PLB[n][:], cur[:, 15:527], 1.0 / W, BQ[n][:, 15:527], ALU.mult, ALU.subtract, curk + K("BQ", n), K("PLB", n))
                if first_half and n == 0:
                    TT = G[gb + 2]
                    tt(DVE, TT[:, 0:16], cur[:, 15:31], ICNT[:, g, :], ALU.mult, curk + K("ICNT"), KG(gb + 2))
                    tt(DVE, PLB[n][:, 0:16], TT[:, 0:16], BQ[n][:, 15:31], ALU.subtract, KG(gb + 2) + K("BQ", n), K("PLB", n))
                mm(pb[bb + 2][:], PW[l][:, g, :], PLB[n][:], True, True, K("PW", l) + K("PLB", n), Kpb(bb + 2))
                SZ = G[gb + 1]
                act(SZ, pb[bb + 1][:], AF.Silu, Kpb(bb + 1), KG(gb + 1))
                stt(YC[:, g, nt], pb[bb + 2][:], col(l, 62 + g), SZ, ALU.mult, ALU.mult,
                    Kpb(bb + 2) + K("COLS") + KG(gb + 1), K("YC", g, n))

        ecnt = {"c": 0}

        def phase_E(l, dc, wg, wbr):
            Ys = [(YA, "YA"), (YB, "YB"), (YC, "YC")]
            for n in range(2):
                nt = slice(n * 512, (n + 1) * 512)
                ai = 2 + (ecnt["c"] // 3) % 2
                ACC, kacc = BQ[ai][:, 0:512], K("BQ", ai)
                for nb in range(3):
                    bp = (ecnt["c"] % 4) * 2
                    si = ecnt["c"] % 2
                    ecnt["c"] += 1
                    S, ks = BQ[si][:, 0:512], K("BQ", si)
                    inproj(bp, wg[nb], n)
                    wt_, k0, wk_ = wbr[nb]
                    Yt, yk = Ys[nb]
                    for kc in range(4):
                        mm(pb[bp + 1][:], wt_[:, k0 + kc, :], Yt[:, kc, nt], kc == 0, kc == 3,
                           wk_ + K(yk, kc, n), Kpb(bp + 1))
                    act(S, pb[bp][:], AF.Sigmoid, Kpb(bp), ks)
                    if nb == 0:
                        tt(DVE, ACC, pb[bp + 1][:], S, ALU.mult, Kpb(bp + 1) + ks, kacc)
                    else:
                        tt(DVE, S, pb[bp + 1][:], S, ALU.mult, Kpb(bp + 1) + ks, ks)
                        if nb == 1:
                            tt(EADD, ACC, ACC, S, ALU.add, kacc + ks, kacc)
                        else:
                            tt(EADD, MG[:, dc, nt], ACC, S, ALU.add, kacc + ks, KG(dc))

        def phase_F(l):
            for t in range(8):
                bp = (t % 4) * 2
                for hf in range(2):
                    for kc in range(8):
                        mm(pb[bp + hf][:], MG[:, kc, t * 128:(t + 1) * 128], WO[:, kc, hf * 512:(hf + 1) * 512], kc == 0, kc == 7,
                           KG(*range(16)), Kpb(bp + hf))
                jk = t % 2
                for hf in range(2):
                    act(BQ[jk][:, 0:512], pb[bp + hf][:], AF.Square, Kpb(bp + hf), K("BQ", jk) + K("SS2", hf),
                        accum_out=SS2[:, hf:hf + 1])
                tt(DVE, SS2[:, 0:1], SS2[:, 0:1], SS2[:, 1:2], ALU.add, K("SS2", [0, 1]), K("SS2", 0))
                act(RS2[:], SS2[:, 0:1], AF.Ln, K("SS2", 0), K("RS2"), scale=1.0 / D_MODEL, bias=NORM_EPS)
                act(RS2[:], RS2[:], AF.Exp, K("RS2"), K("RS2"), scale=-0.5)
                for hf in range(2):
                    T = BQ[2 + hf][:, 0:512]
                    hs = slice(hf * 512, (hf + 1) * 512)
                    tt(DVE, T, pb[bp + hf][:], GPOST[:, hs], ALU.mult, Kpb(bp + hf) + K("GPOST"), K("BQ", 2 + hf))
                    stt(XS[:, t, hs], T, RS2[:], XS[:, t, hs], ALU.mult, ALU.add, K("BQ", 2 + hf) + K("RS2") + K("XS", t), K("XS", t))

        stages = []

        stage_names = []

        def add_stage(load, compute, name="?"):
            stages.append((load, compute))
            stage_names.append(name)

        for s in range(n_seq):
            for hf in range(n_half):
                first_half = hf == 0
                base = s * SEQ + hf * HALF

                def pass_begin(base=base, first_half=first_half):
                    P.dma(lambda e: e.dma_start(out=XS[:], in_=x_d[base:base + HALF, :].rearrange("(t p) d -> p t d", p=128)),
                          writes=K("XS", list(range(8))), dkey="xin")
                    if first_half:
                        for l in range(2):
                            P.add(POOL, lambda e, l=l: e.memset(S32[l][:], 0.0), (), K("S32", l, [0, 1, 2, 3]))
                            P.add(POOL, lambda e, l=l: e.memset(CAR[l][:], 0.0), (), K("CAR", l, list(range(18))))
                            P.add(POOL, lambda e, l=l: e.memset(CCAR[l][:], 0.0), (), K("CCAR", l, [0, 1, 2, 3]))
                            P.add(POOL, lambda e, l=l: e.memset(PCAR[l][:], 0.0), (), K("PCAR", l, [0, 1, 2, 3]))

                for l in range(n_layers):
                    h = {}

                    def ld0(l=l, h=h):
                        ws = load_in_chunks(l, [2048])
                        if l == 1:
                            s_ = wload(vdn_d.rearrange("(k p) n -> p k n", p=128), ncol=32)
                            ws.append(wcast(s_, ncol=32))
                        h["b0"] = ws

                    def c0(l=l, h=h, is_first=(l == 0), pb_=pass_begin):
                        if is_first:
                            pb_()
                        P.dma(lambda e: e.dma_start(out=GPRE[:], in_=gpre_d[l:l + 1, :].broadcast_to([128, D_MODEL])),
                              writes=K("GPRE"), dkey="gpre")
                        P.dma(lambda e: e.dma_start(out=GPOST[:], in_=gpost_d[l:l + 1, :].broadcast_to([128, D_MODEL])),
                              writes=K("GPOST"), dkey="gpost")
                        phase_A(l)
                        phase_B0(l, h["b0"])

                    add_stage(ld0, c0, "A")
                    for j in range(4):
                        def ldB(l=l, j=j, h=h):
                            h["B", j] = load_in_chunks(l, [q * 512 + j * 128 for q in range(4)])
                        add_stage(ldB, lambda l=l, j=j, h=h, fh=first_half: phase_B(l, j, h["B", j], fh, h.get(("B", j + 1))), "B")
                    def ld_wo(l, ec):
                        wov = w_out_d[l].rearrange("(k p) e -> p k e", p=128)
                        s_ = wload(wov[:, :, ec * 128:(ec + 1) * 128])
                        wcast(s_, dst=WO[:, :, ec * 128:(ec + 1) * 128], dst_keys=KG(*range(8, 16)))

                    for j in range(4):
                        def ldC(l=l, j=j, h=h):
                            h["C", j] = load_in_chunks(l, [2176 + q * 512 + j * 128 for q in range(4)])
                            if j >= 1:
                                ld_wo(l, j - 1)
                        add_stage(ldC, lambda l=l, j=j, h=h: phase_C(l, j, h["C", j]), "C")
                    for g in range(4):
                        def ldD(l=l, g=g, h=h):
                            h["D", g] = load_in_chunks(l, [4224 + q * 512 + g * 128 for q in range(2)])
                            ld_wo(l, 3 + g)
                        add_stage(ldD, lambda l=l, g=g, h=h, fh=first_half: phase_D(l, g, h["D", g], fh), "D")
                    for dc in range(8):
                        def ldE(l=l, dc=dc, h=h):
                            wg = load_in_chunks(l, [5248 + nb * 1024 + dc * 128 for nb in range(3)])
                            brv = w_br_d[l].rearrange("n (k p) d -> n p k d", p=128)
                            s01 = wload(brv[0][:, :, dc * 128:(dc + 1) * 128], nk=4, k0=0)
                            wload(brv[1][:, :, dc * 128:(dc + 1) * 128], nk=4, k0=4, same_stage=True)
                            w01 = wcast(s01)
                            s2 = wload(brv[2][:, :, dc * 128:(dc + 1) * 128], nk=4, k0=0)
                            wcast(s2, nk=4, dst=WBR2[dc % 2][:], dst_keys=K("wbr2", dc % 2))
                            if dc == 0:
                                ld_wo(l, 7)
                            h["E", dc] = (wg, [(WBF[w01], 0, K("wb", w01)), (WBF[w01], 4, K("wb", w01)),
                                               (WBR2[dc % 2], 0, K("wbr2", dc % 2))])
                        add_stage(ldE, lambda l=l, dc=dc, h=h: phase_E(l, dc, *h["E", dc]), "E")

                    def ldF(l=l):
                        pass

                    def cF(l=l, last=(l == n_layers - 1), base=base):
                        phase_F(l)
                        if last:
                            P.dma(lambda e: e.dma_start(out=y_d[base:base + HALF, :].rearrange("(t p) d -> p t d", p=128), in_=XS[:]),
                                  reads=K("XS", list(range(8))), dkey="xout")

                    add_stage(ldF, cF, "F")

        if dbg:
            pass
        global _LASTP
        _LASTP = P
        del _MARKS[:]
        stages[0][0]()
        for i, (ld, comp) in enumerate(stages):
            if i + 1 < len(stages):
                stages[i + 1][0]()
            _MARKS.append((stage_names[i], len(P.ops)))
            comp()
            if dbg and i == 12:
                for bi, (Yt, yk) in enumerate([(YA, "YA"), (YB, "YB"), (YC, "YC")]):
                    for jj in range(4):
                        for n in range(2):
                            cp(DVE, G[0], Yt[:, jj, n * 512:(n + 1) * 512], K(yk, jj, n), KG(0))
                            P.dma(lambda e, bi=bi, jj=jj, n=n: e.dma_start(out=dbg_d[bi, :, jj, n * 512:(n + 1) * 512], in_=G[0]),
                                  reads=KG(0), dkey="dbg")
        stats = P.emit(nc, sems, dsem)
    return nc, stats


def _colpack(v):
    v = np.asarray(v, np.float32).reshape(-1)
    n = (v.size + 127) // 128
    out = np.zeros((n * 128,), np.float32)
    out[:v.size] = v
    return out.reshape(n, 128).T


def host_consts():
    s = np.arange(128)[:, None]
    t = np.arange(128)[None, :]
    strict = (s < t).astype(np.float32)
    incl = (s <= t).astype(np.float32)
    mask4 = np.concatenate([strict, incl, strict, incl], axis=1)
    maskl = (t < s).astype(np.float32)
    bones = np.zeros((128, 128), np.float32)
    bones[:64, :64] = 1.0
    bones[64:, 64:] = 1.0
    rmask = np.ones((128, 512), np.float32)
    rmask[:, ::128] = 0.0
    icnt = np.zeros((128, 4, 16), np.float32)
    for g, w in enumerate(POOL_WINDOWS):
        icnt[:, g, :] = 1.0 / np.minimum(np.arange(1, 17), w)
    return dict(identf=np.eye(128, dtype=np.float32), mask4=mask4, maskl=maskl, bones=bones, rmask=rmask,
                icnt=icnt.reshape(128, 64))


def host_pack(inp):
    f = lambda a: np.ascontiguousarray(np.asarray(a, np.float32))
    cols = []
    for l in range(2):
        cl = [_colpack(inp["mu_shift"][l])]
        cl.append(_colpack(inp["vres_mu"][0]) if l == 1 else np.zeros((128, 1), np.float32))
        for nm in ["rwkv_w0", "rwkv_a0", "rwkv_k_k", "rwkv_k_a", "rwkv_r_k", "rwkv_gn_w", "rwkv_gn_b"]:
            cl.append(_colpack(inp[nm][l]))
        cl.append(_colpack(inp["vres_v0"][0]) if l == 1 else np.zeros((128, 4), np.float32))
        for i in range(3):
            cl.append(_colpack(inp["conv_w"][l][i]))
        cl.append(_colpack(inp["pool_scale"][l]))
        c = np.concatenate(cl, axis=1)
        assert c.shape == (128, NCL), c.shape
        cols.append(c)
    shared = dict(
        w_in=f(inp["w_in"]), w_branch=f(inp["w_branch"]), w_out=f(inp["w_out"]),
        vres_down=f(inp["vres_down"][0]),
        lw=f(np.concatenate([inp["rwkv_w_up"], inp["rwkv_a_up"]], axis=1)),
        vres_up=f(inp["vres_up"][0]), pool_w=f(inp["pool_w"]),
        cols=f(np.concatenate(cols, axis=1)),
        gpre=f(inp["pre_norm_w"]), gpost=f(inp["post_norm_w"]),
    )
    shared.update(host_consts())
    return shared


_CACHE = {}
_MARKS = []
_LASTP = None


def kernel(**inputs):
    x = np.asarray(inputs["x"], np.float32)
    B = x.shape[0]
    ncore = 8
    per = B // ncore
    shared = host_pack(inputs)
    if "nc" not in _CACHE:
        _CACHE["nc"] = build_program(n_seq=per)[0]
    nc = _CACHE["nc"]
    in_maps = []
    for i in range(ncore):
        m = dict(shared)
        m["x"] = np.ascontiguousarray(x[i * per:(i + 1) * per].reshape(per * SEQ, D_MODEL))
        in_maps.append(m)
    res = run_bass_kernel_spmd(nc, in_maps, core_ids=list(range(ncore)))
    out = np.concatenate([r["y"].reshape(per, SEQ, D_MODEL) for r in res.results], axis=0)
    return out.astype(np.float32)
```

```python
import itertools
import math
from contextlib import ExitStack

import numpy as np
import concourse.bass as bass
import concourse.mybir as mybir
from concourse.bass_utils import run_bass_kernel_spmd

F32 = mybir.dt.float32
BF16 = mybir.dt.bfloat16
ALU = mybir.AluOpType
AF = mybir.ActivationFunctionType
AX = mybir.AxisListType

PE, ACT, DVE, POOL, SP = "pe", "act", "dve", "pool", "sp"
COMPUTE = (PE, ACT, DVE, POOL)

D_MODEL = 1024
SEQ = 2048
N_IN = 8320
HALF = 1024
NCL = 66
C0 = -math.exp(-0.5)
GN_EPS = 64 * 1e-5
NORM_EPS = 1e-6
POOL_WINDOWS = (2, 4, 8, 16)
import os as _os
ESHIFT = int(_os.environ.get("KB_ESHIFT", "1"))
EADD = POOL
ABSPLIT = 0


def K(name, *idx):
    dims = []
    for i in idx:
        if isinstance(i, (list, tuple, range)):
            dims.append(list(i))
        else:
            dims.append([i])
    return [(name,) + t for t in itertools.product(*dims)]


class Op:
    __slots__ = ("eng", "fn", "deps", "sig", "idx", "is_dma", "dkey", "dval", "waits")


class Prog:
    def __init__(self, batch_keys=()):
        self.ops = []
        self.last_w = {}
        self.readers = {}
        self.batch_keys = set(batch_keys)
        self._cap = None

    def begin_capture(self):
        self._cap = []

    def end_capture(self):
        c, self._cap = self._cap, None
        return c

    def replay(self, item):
        return self.add(*item)

    def add(self, eng, fn, reads=(), writes=(), is_dma=False, dkey=None):
        if self._cap is not None:
            self._cap.append((eng, fn, list(reads), list(writes), is_dma, dkey))
            return None
        op = Op()
        op.eng, op.fn, op.is_dma, op.dkey = eng, fn, is_dma, dkey
        op.sig, op.dval, op.waits = False, 0, []
        op.idx = len(self.ops)
        if eng != PE:
            pbk = [k for k in reads if k[0] == "pb"]
            if pbk:
                reads = [k for k in reads if k[0] != "pb"]
                writes = list(writes) + pbk
        deps = set()
        for k in reads:
            w = self.last_w.get(k)
            if w is not None:
                deps.add(w)
        for k in writes:
            w = self.last_w.get(k)
            if w is not None:
                deps.add(w)
            for r in self.readers.get(k, ()):
                deps.add(r)
        deps.discard(op.idx)
        op.deps = sorted(deps)
        for k in writes:
            self.last_w[k] = op.idx
            self.readers[k] = []
        for k in reads:
            self.readers.setdefault(k, []).append(op.idx)
        self.ops.append(op)
        return op

    def dma(self, fn, reads=(), writes=(), dkey="dma", eng=SP):
        return self.add(eng, fn, reads, writes, is_dma=True, dkey=dkey)

    def emit(self, nc, sems, dma_sems):
        ops = self.ops
        dcnt = {}
        for op in ops:
            if op.is_dma:
                dcnt[op.dkey] = dcnt.get(op.dkey, 0) + 16
                op.dval = dcnt[op.dkey]
        need = [False] * len(ops)
        waited = {}
        wl = [None] * len(ops)
        for op in ops:
            w = {}
            for d in op.deps:
                dop = ops[d]
                if dop.is_dma:
                    key = ("d", dop.dkey)
                    r = dcnt[dop.dkey] if dop.dkey in self.batch_keys else dop.dval
                else:
                    if dop.eng == PE and op.eng == PE and not op.is_dma:
                        continue
                    key = ("c", dop.eng)
                    r = d
                if r > w.get(key, -1):
                    w[key] = r
            ew = waited.setdefault(op.eng, {})
            lst = []
            for key, r in w.items():
                if ew.get(key, -1) >= r:
                    continue
                ew[key] = r
                lst.append((key, r))
                if key[0] == "c":
                    need[r] = True
            wl[op.idx] = lst
        cnt = {e: 0 for e in COMPUTE}
        val = [0] * len(ops)
        for op in ops:
            if not op.is_dma and need[op.idx]:
                cnt[op.eng] += 1
                val[op.idx] = cnt[op.eng]
                op.sig = True
        for op in ops:
            op.waits = [(key, (val[r] if key[0] == "c" else r)) for key, r in wl[op.idx]]
        self.final_cnt = (dict(cnt), dict(dcnt))
        by_eng = {}
        for op in ops:
            by_eng.setdefault(op.eng, []).append(op)

        def run(engname, e):
            for op in by_eng.get(engname, []):
                for key, v in op.waits:
                    s = dma_sems[key[1]] if key[0] == "d" else sems[key[1]]
                    e.wait_ge(s, v)
                ins = op.fn(e)
                if op.is_dma:
                    ins.then_inc(dma_sems[op.dkey], 16)
                elif op.sig:
                    ins.then_inc(sems[op.eng], 1)

        with nc.Block() as block:
            @block.tensor
            def _(e):
                run(PE, e)

            @block.scalar
            def _(e):
                run(ACT, e)

            @block.vector
            def _(e):
                run(DVE, e)

            @block.gpsimd
            def _(e):
                run(POOL, e)

            @block.sync
            def _(e):
                run(SP, e)
                for k, v in dcnt.items():
                    e.wait_ge(dma_sems[k], v)
        return {e: len(v) for e, v in by_eng.items()}


def build_program(n_seq=2, n_half=2, n_layers=2, dbg=False):
    nc = bass.Bass("TRN2", target_bir_lowering=False)
    NTOK = n_seq * SEQ

    def din(name, shape):
        return nc.dram_tensor(name, shape, F32, kind="ExternalInput").ap()

    x_d = din("x", [NTOK, D_MODEL])
    w_in_d = din("w_in", [2, D_MODEL, N_IN])
    w_br_d = din("w_branch", [2, 3, 512, D_MODEL])
    w_out_d = din("w_out", [2, D_MODEL, D_MODEL])
    vdn_d = din("vres_down", [D_MODEL, 32])
    lw_d = din("lw", [2, 128, 512])
    vup_d = din("vres_up", [32, 512])
    pw_d = din("pool_w", [2, 4, 128, 128])
    cols_d = din("cols", [128, 2 * NCL])
    gpre_d = din("gpre", [2, D_MODEL])
    gpost_d = din("gpost", [2, D_MODEL])
    identf_d = din("identf", [128, 128])
    mask4_d = din("mask4", [128, 512])
    maskl_d = din("maskl", [128, 128])
    bones_d = din("bones", [128, 128])
    rmask_d = din("rmask", [128, 512])
    icnt_d = din("icnt", [128, 64])
    y_d = nc.dram_tensor("y", [NTOK, D_MODEL], F32, kind="ExternalOutput").ap()
    if dbg:
        dbg_d = nc.dram_tensor("dbg", [3, 128, 4, HALF], F32, kind="ExternalOutput").ap()

    NST, NWB = 3, 8
    dkeys = ["const", "xin", "xout", "gpre", "gpost", "dbg"] + ["st%d" % i for i in range(NST)]
    P = Prog(batch_keys=["const"])

    with ExitStack() as es:
        def sb(name, shape, dt=F32):
            return es.enter_context(nc.sbuf_tensor(name, shape, dt))

        XS = sb("XS", [128, 8, D_MODEL])
        HT = sb("HT", [128, 8, HALF], BF16)
        YA = sb("YA", [128, 4, HALF], BF16)
        YB = sb("YB", [128, 4, HALF], BF16)
        YC = sb("YC", [128, 4, HALF], BF16)
        VF = sb("VF", [128, 4, HALF], BF16)
        ARENA = sb("ARENA", [128, 17 * 512])
        G = [ARENA[:, g * 512:(g + 1) * 512] for g in range(17)]
        MG = ARENA[:, 0:4096].bitcast(BF16).rearrange("p (k t) -> p k t", k=8)
        WO = ARENA[:, 4096:8192].bitcast(BF16).rearrange("p (k t) -> p k t", k=8)
        BQ = [sb("BQ%d" % i, [128, 528]) for i in range(4)]
        ART = sb("ART", [128, 4, 2, 128], BF16)
        BKT = sb("BKT", [128, 4, 2, 128], BF16)
        BKTOK = sb("BKTOK", [128, 4, 2, 128], BF16)
        VTOK = sb("VTOK", [128, 4, 128], BF16)
        AB = [sb("AB%d" % u, [128, 512], BF16) for u in range(8)]
        NN = [sb("NN%d" % u, [128, 128], BF16) for u in range(8)]
        PTP = [sb("PTP%d" % p_, [128, 256], BF16) for p_ in range(4)]
        PT = [PTP[u // 2][:, (u % 2) * 128:(u % 2 + 1) * 128] for u in range(8)]
        MMP = [sb("MMP%d" % p_, [128, 512], BF16) for p_ in range(4)]
        XSB = sb("XSB", [128, 2, 64], BF16)
        USB = sb("USB", [128, 2, 64], BF16)
        STB = [sb("STB%d" % i, [128, 64], BF16) for i in range(2)]
        STG = sb("STG", [128, 64])
        S32 = [sb("S32_%d" % l, [128, 4, 64]) for l in range(2)]
        YN = sb("YN", [128, 4, 128])
        BONX = sb("BONX", [128, 512])
        ZGX = sb("ZGX", [128, 512])
        JNK = sb("JNK", [128, 64])
        GC = sb("GC", [128, 4])
        SUM8 = sb("SUM8", [128, 8])
        SSQ8 = sb("SSQ8", [128, 8])
        M8 = sb("M8", [128, 8])
        V8 = sb("V8", [128, 8])
        SS1 = sb("SS1", [128, 2])
        RST = sb("RST", [128, 2])
        SS2 = sb("SS2", [128, 2])
        RS2 = sb("RS2", [128, 1])
        CAR = [sb("CAR%d" % l, [128, 18]) for l in range(2)]
        CCAR = [sb("CCAR%d" % l, [128, 4, 2]) for l in range(2)]
        PCAR = [sb("PCAR%d" % l, [128, 4, 15]) for l in range(2)]
        LD = sb("LD", [128, HALF], BF16)
        PLBALL = sb("PLBALL", [128, 2, 512], BF16)
        PLB = [PLBALL[:, i, :] for i in range(2)]
        VD = PLBALL[0:32].rearrange("p n t -> p (n t)")
        WBR2 = [sb("WBR2_%d" % i, [128, 4, 128], BF16) for i in range(2)]
        IDF = sb("IDF", [128, 128])
        IDB = sb("IDB", [128, 128], BF16)
        MASK4 = sb("MASK4", [128, 512], BF16)
        MASKL = sb("MASKL", [128, 128], BF16)
        BONES = sb("BONES", [128, 128])
        RMASK = sb("RMASK", [128, 512], BF16)
        ICNT = sb("ICNT", [128, 4, 16])
        COLS = sb("COLS", [128, 2 * NCL])
        DER = sb("DER", [128, 2 * NCL])
        LW = [sb("LW%d" % l, [128, 512], BF16) for l in range(2)]
        VU = sb("VU", [32, 512], BF16)
        PW = [sb("PW%d" % l, [128, 4, 128], BF16) for l in range(2)]
        GPRE = sb("GPRE", [128, D_MODEL])
        GPOST = sb("GPOST", [128, D_MODEL])
        STAGE = [sb("STAGE%d" % i, [128, 8, 128]) for i in range(NST)]
        WBF = [sb("WBF%d" % i, [128, 8, 128], BF16) for i in range(NWB)]
        pb = [es.enter_context(nc.psum_tensor("pb%d" % i, [128, 512], F32)) for i in range(8)]

        sems = {e: es.enter_context(nc.semaphore("s_" + e)) for e in COMPUTE}
        dsem = {k: es.enter_context(nc.semaphore("d_" + k)) for k in dkeys}

        def act(out, in_, func, reads, writes, **kw):
            P.add(ACT, lambda e: e.activation(out=out, in_=in_, func=func, **kw), reads, writes)

        def tt(eng, out, in0, in1, op, reads, writes):
            P.add(eng, lambda e: e.tensor_tensor(out=out, in0=in0, in1=in1, op=op), reads, writes)

        def ts(eng, out, in0, s1, s2, op0, op1, reads, writes):
            if s2 is None:
                P.add(eng, lambda e: e.tensor_scalar(out=out, in0=in0, scalar1=s1, scalar2=None, op0=op0), reads, writes)
            else:
                P.add(eng, lambda e: e.tensor_scalar(out=out, in0=in0, scalar1=s1, scalar2=s2, op0=op0, op1=op1), reads, writes)

        def stt(out, in0, scalar, in1, op0, op1, reads, writes):
            P.add(DVE, lambda e: e.scalar_tensor_tensor(out=out, in0=in0, scalar=scalar, in1=in1, op0=op0, op1=op1),
                  reads, writes)

        def cp(eng, out, in_, reads, writes):
            if eng == ACT:
                act(out, in_, AF.Copy, reads, writes)
            else:
                P.add(eng, lambda e: e.tensor_copy(out=out, in_=in_), reads, writes)

        def mm(out, lhsT, rhs, start, stop, reads, writes):
            P.add(PE, lambda e: e.matmul(out, lhsT=lhsT, rhs=rhs, start=start, stop=stop), reads, writes)

        def tr(out, in_, ident, reads, writes):
            P.add(PE, lambda e: e.transpose(out=out, in_=in_, identity=ident), reads, writes)

        def col(l, c):
            return COLS[:, l * NCL + c: l * NCL + c + 1]

        def dcol(l, c):
            return DER[:, l * NCL + c: l * NCL + c + 1]

        KG = lambda *g: K("G", list(g))
        Kpb = lambda *b: K("pb", list(b))

        wstate = {"st": 0, "wb": 0}

        def wload(src, nk=8, ncol=128, dst=None, dst_keys=None, k0=0, same_stage=False):
            if same_stage:
                s = (wstate["st"] - 1) % NST
            else:
                s = wstate["st"] % NST
                wstate["st"] += 1
            P.dma(lambda e: e.dma_start(out=STAGE[s][:, k0:k0 + nk, 0:ncol], in_=src),
                  writes=K("st", s, list(range(k0, k0 + nk))), dkey="st%d" % s)
            return s

        def wcast(s, nk=8, ncol=128, dst=None, dst_keys=None, eng=POOL):
            if dst is None:
                w = wstate["wb"] % NWB
                wstate["wb"] += 1
                dst = WBF[w][:, 0:nk, 0:ncol]
                dst_keys = K("wb", w)
            else:
                w = None
            src_ = STAGE[s][:, 0:nk, 0:ncol]
            if ncol == 128 and w is not None:
                dst = WBF[w][:, 0:nk, :].rearrange("p k n -> p (k n)")
                src_ = STAGE[s][:, 0:nk, :].rearrange("p k n -> p (k n)")
            cp(eng, dst, src_, K("st", s, list(range(nk))), dst_keys)
            return w

        def w_in_chunk(l, c0, ncol=128):
            return w_in_d[l].rearrange("(k p) n -> p k n", p=128)[:, :, c0:c0 + ncol]

        def load_in_chunks(l, c0s):
            ws = []
            for c0 in c0s:
                s = wload(w_in_chunk(l, c0))
                ws.append(wcast(s))
            return ws

        def cdma(out, in_, wkeys):
            P.dma(lambda e: e.dma_start(out=out, in_=in_), writes=wkeys, dkey="const")

        cdma(IDF[:], identf_d, K("IDF"))
        cdma(G[5][:, 0:512], mask4_d, KG(5))
        cdma(G[6][:, 0:128], maskl_d, KG(6))
        cdma(BONES[:], bones_d, K("BONES"))
        cdma(G[7][:, 0:512], rmask_d, KG(7))
        cdma(ICNT[:].rearrange("p g t -> p (g t)"), icnt_d, K("ICNT"))
        cdma(COLS[:], cols_d, K("COLS"))
        cdma(G[0][:, 0:512], lw_d[0], KG(0))
        cdma(G[1][:, 0:512], lw_d[1], KG(1))
        cdma(G[2][0:32, 0:512], vup_d, KG(2))
        cdma(G[3].rearrange("p (g d) -> p g d", g=4), pw_d[0].rearrange("g c d -> c g d"), KG(3))
        cdma(G[4].rearrange("p (g d) -> p g d", g=4), pw_d[1].rearrange("g c d -> c g d"), KG(4))
        cp(POOL, LW[0][:], G[0], KG(0), K("LW", 0))
        cp(POOL, LW[1][:], G[1], KG(1), K("LW", 1))
        cp(POOL, VU[:], G[2][0:32, :], KG(2), K("VU"))
        cp(POOL, PW[0][:].rearrange("p g d -> p (g d)"), G[3], KG(3), K("PW", 0))
        cp(POOL, PW[1][:].rearrange("p g d -> p (g d)"), G[4], KG(4), K("PW", 1))
        cp(POOL, IDB[:], IDF[:], K("IDF"), K("IDB"))
        cp(POOL, MASK4[:], G[5], KG(5), K("MASK4"))
        cp(POOL, MASKL[:], G[6][:, 0:128], KG(6), K("MASKL"))
        cp(POOL, RMASK[:], G[7], KG(7), K("RMASK"))
        ts(DVE, DER[:], COLS[:], -1.0, 1.0, ALU.mult, ALU.add, K("COLS"), K("DER"))

        def phase_A(l):
            for t in range(8):
                p = t % 2
                sq = ARENA[:, 0:1024]
                xsc = ARENA[:, 1024 + p * 1024: 2048 + p * 1024]
                kx = KG(2 + 2 * p, 3 + 2 * p)
                act(sq, XS[:, t, :], AF.Square, K("XS", t), KG(0, 1) + K("SS1", p), accum_out=SS1[:, p:p + 1])
                act(RST[:, p:p + 1], SS1[:, p:p + 1], AF.Ln, K("SS1", p), K("RST", p), scale=1.0 / D_MODEL, bias=NORM_EPS)
                act(RST[:, p:p + 1], RST[:, p:p + 1], AF.Exp, K("RST", p), K("RST", p), scale=-0.5)
                stt(xsc, XS[:, t, :], RST[:, p:p + 1], GPRE[:], ALU.mult, ALU.mult,
                    K("XS", t) + K("RST", p) + K("GPRE"), kx)
                b0 = 2 * p
                for kc in range(8):
                    tr(pb[b0 + kc // 4][:, (kc % 4) * 128:(kc % 4 + 1) * 128], xsc[:, kc * 128:(kc + 1) * 128], IDF[:],
                       kx + K("IDF"), Kpb(b0 + kc // 4))
                cp(ACT, HT[:, 0:4, t * 128:(t + 1) * 128], pb[b0][:].rearrange("p (k t) -> p k t", k=4), Kpb(b0), K("HT", t))
                cp(DVE, HT[:, 4:8, t * 128:(t + 1) * 128], pb[b0 + 1][:].rearrange("p (k t) -> p k t", k=4), Kpb(b0 + 1), K("HT", t))

        def inproj(bank, w, n, M=128):
            for kc in range(8):
                mm(pb[bank][0:M, :], WBF[w][:, kc, 0:M], HT[:, kc, n * 512:(n + 1) * 512], kc == 0, kc == 7,
                   K("wb", w) + K("HT", list(range(4 * n, 4 * n + 4))), Kpb(bank))

        def shift(l, ci, bank, bq, dst, dkeys, M=128):
            cp(ACT, BQ[bq][0:M, 0:1], CAR[l][0:M, ci:ci + 1], K("CAR", l, ci), K("BQ", bq))
            act(BQ[bq][0:M, 1:513], pb[bank][0:M, :], AF.Copy, Kpb(bank) + K("COLS"), K("BQ", bq), scale=col(l, ci)[0:M])
            cp(ACT, CAR[l][0:M, ci:ci + 1], BQ[bq][0:M, 512:513], K("BQ", bq), K("CAR", l, ci))
            stt(dst, pb[bank][0:M, :], dcol(l, ci)[0:M], BQ[bq][0:M, 0:512], ALU.mult, ALU.add,
                Kpb(bank) + K("DER") + K("BQ", bq), dkeys)

        def phase_B0(l, ws):
            for n in range(2):
                nt = slice(n * 512, (n + 1) * 512)
                inproj(n, ws[0], n)
                shift(l, 16, n, 0, G[0], KG(0))
                act(LD[0:64, nt], G[0][0:64, :], AF.Tanh, KG(0), K("LD", n, 0))
                cp(ACT, LD[64:128, nt], G[0][64:128, :], KG(0), K("LD", n, 1))
                if l == 1:
                    inproj(2 + n, ws[1], n, M=32)
                    shift(l, 17, 2 + n, 1, G[1][0:32, :], KG(1), M=32)
                    cp(ACT, VD[0:32, nt], G[1][0:32, :], KG(1), K("PLB", n))

        def inproj_ops(bank, w, n):
            ops_ = []
            for kc in range(8):
                ops_.append(lambda bank=bank, w=w, n=n, kc=kc: mm(
                    pb[bank][:, :], WBF[w][:, kc, :], HT[:, kc, n * 512:(n + 1) * 512], kc == 0, kc == 7,
                    K("wb", w) + K("HT", list(range(4 * n, 4 * n + 4))), Kpb(bank)))
            return ops_

        bstate = {"pre": None, "shifted": None, "gn": None}

        def phase_B(l, j, ws, first_half, ws_next=None):
            jc = slice(j * 128, (j + 1) * 128)
            R32, K32, V32, Z32, SG, A32, KK, KP, CS, EP, EN, BON, T0, T1, T2, T3, T4 = G
            pb5b = pb[5][:].bitcast(BF16)
            for n in range(2):
                nt = slice(n * 512, (n + 1) * 512)
                if bstate["pre"] != (l, j, n):
                    for q in range(4):
                        inproj(q, ws[q], n)
                bstate["pre"] = None
                if n == 0:
                    nxt_w, nxt_key = ws, (l, j, 1)
                    nxt_n = 1
                elif ws_next is not None:
                    nxt_w, nxt_key = ws_next, (l, j + 1, 0)
                    nxt_n = 0
                else:
                    nxt_w, nxt_key = None, None
                fill = []
                if nxt_w is not None:
                    for q in range(4):
                        fill.extend(inproj_ops(q, nxt_w[q], nxt_n))

                def emit_fill(k):
                    for _ in range(k):
                        if fill:
                            fill.pop(0)()
                mm(pb[4][:], LW[l][0:64, jc], LD[0:64, nt], True, True, K("LW", l) + K("LD", n, 0), Kpb(4))
                mm(pb[5][:], LW[l][64:128, jc], LD[64:128, nt], True, True, K("LW", l) + K("LD", n, 1), Kpb(5))
                if l == 1:
                    mm(pb[6][:], VU[0:32, jc], VD[0:32, nt], True, True, K("VU") + K("PLB", n), Kpb(6))
                if bstate["shifted"] != (l, j, n):
                    for q in range(4):
                        shift(l, q * 4 + j, q, q, G[q], KG(q))
                bstate["shifted"] = None
                BONp, kbon = (BON, KG(11)) if n == 0 else (BONX[:], K("BONX"))
                ZGp, kzg = (T4, KG(16)) if n == 0 else (ZGX[:], K("ZGX"))
                P.begin_capture()
                act(SG, pb[4][:], AF.Sigmoid, Kpb(4) + K("COLS"), KG(4), bias=col(l, 18 + j))
                act(A32, pb[5][:], AF.Sigmoid, Kpb(5) + K("COLS"), KG(5), bias=col(l, 22 + j))
                act(ZGp, Z32, AF.Sigmoid, KG(3), kzg)
                tt(POOL, ZGp, ZGp, Z32, ALU.mult, kzg + KG(3), kzg)
                if l == 0:
                    cp(POOL, VF[:, j, nt], V32, KG(2), K("VF", j, n))
                else:
                    act(T0, pb[6][:], AF.Sigmoid, Kpb(6) + K("COLS"), KG(12), bias=col(l, 46 + j))
                    tt(DVE, T1, VF[:, j, nt], V32, ALU.subtract, K("VF", j, n) + KG(2), KG(13))
                    tt(DVE, T1, T1, T0, ALU.mult, KG(13, 12), KG(13))
                    tt(DVE, V32, V32, T1, ALU.add, KG(2, 13), KG(2))
                act(T2, K32, AF.Square, KG(1) + K("COLS"), KG(14), scale=col(l, 26 + j))
                mm(pb[0][:], BONES[:], T2, True, True, K("BONES") + KG(14), Kpb(0))
                act(T2, pb[0][:], AF.Ln, Kpb(0), KG(14), bias=1e-24)
                act(T2, T2, AF.Exp, KG(14), KG(14), scale=-0.5)
                stt(KK, K32, col(l, 26 + j), T2, ALU.mult, ALU.mult, KG(1, 14) + K("COLS"), KG(6))
                act(T3, A32, AF.Identity, KG(5) + K("COLS") + K("DER"), KG(15), scale=col(l, 30 + j), bias=dcol(l, 30 + j))
                tt(DVE, KP, K32, T3, ALU.mult, KG(1, 15), KG(7))
                tt(DVE, T3, KK, A32, ALU.mult, KG(6, 5, 7), KG(15))
                P.add(DVE, lambda e: e.tensor_tensor_scan(out=CS, data0=RMASK[:], data1=SG, initial=0.0,
                                                          op0=ALU.mult, op1=ALU.add), K("RMASK") + KG(4), KG(8))
                act(EP, CS, AF.Exp, KG(8), KG(9), scale=C0)
                act(EN, CS, AF.Exp, KG(8), KG(10), scale=-C0)
                tt(DVE, T1, CS, SG, ALU.subtract, KG(8, 4), KG(13))
                act(T1, T1, AF.Exp, KG(13), KG(13), scale=C0)
                cp(ACT, GC[:], EP[:, 127::128], KG(9), K("GC"))
                v4 = lambda a: a.rearrange("p (c t) -> p c t", c=4)
                tt(POOL, ART[:, :, 1, :], v4(R32), v4(EP), ALU.mult, KG(0, 9), K("ART", 1))
                stt(ART[:, :, 0, :], v4(KK), -1.0, v4(T1), ALU.mult, ALU.mult, KG(6, 13), K("ART", 0))
                tt(POOL, BKT[:, :, 0, :], v4(T3), v4(EN), ALU.mult, KG(15, 10), K("BKT", 0))
                tt(DVE, BKT[:, :, 1, :], v4(KP), v4(EN), ALU.mult, KG(7, 10), K("BKT", 1))
                ts(POOL, T2, R32, col(l, 34 + j), 1.0, ALU.mult, ALU.mult, KG(0) + K("COLS"), KG(14))
                tt(POOL, T2, T2, KP, ALU.mult, KG(14, 7), KG(14))
                mm(pb[1][:], BONES[:], T2, True, True, K("BONES") + KG(14), Kpb(1))
                tt(DVE, BONp, pb[1][:], V32, ALU.mult, Kpb(1) + KG(2), kbon)
                for c in range(4):
                    for i in range(2):
                        tr(pb5b[:, (c * 2 + i) * 128:(c * 2 + i + 1) * 128], BKT[:, c, i, :], IDB[:],
                           K("BKT", i) + K("IDB"), Kpb(5))
                cp(ACT, BKTOK[:].rearrange("p c i k -> p (c i k)"), pb5b, Kpb(5), K("BKTOK"))
                for c in range(4):
                    tr(pb[6][:, c * 128:(c + 1) * 128], V32[:, c * 128:(c + 1) * 128], IDF[:], KG(2) + K("IDF"), Kpb(6))
                cp(DVE, VTOK[:].rearrange("p c v -> p (c v)"), pb[6][:], Kpb(6), K("VTOK"))
                pre_ops = P.end_capture()
                gn_ops = bstate["gn"] or []
                bstate["gn"] = None
                for i_, it_ in enumerate(pre_ops):
                    P.replay(it_)
                    if gn_ops:
                        P.replay(gn_ops.pop(0))
                for it_ in gn_ops:
                    P.replay(it_)
                units = [(c, h) for c in range(4) for h in range(2)]
                for u, (c, h) in enumerate(units):
                    po = 64 * h
                    ar = ART[po:po + 64, c].rearrange("p a t -> p (a t)")
                    mm(pb[u][:, 0:256], BKT[po:po + 64, c, 0, :], ar, True, True, K("BKT", 0) + K("ART", [0, 1]), Kpb(u))
                    mm(pb[u][:, 256:512], BKT[po:po + 64, c, 1, :], ar, True, True, K("BKT", 1) + K("ART", [0, 1]), Kpb(u))
                for u in range(8):
                    if ABSPLIT:
                        tt(DVE, AB[u][:, 0:256], pb[u][:, 0:256], MASK4[:, 0:256], ALU.mult, Kpb(u) + K("MASK4"), K("AB", u, 0))
                        cp(ACT, AB[u][:, 256:512], pb[u][:, 256:512], Kpb(u), K("AB", u, 1))
                    else:
                        tt(DVE, AB[u][:], pb[u][:], MASK4[:], ALU.mult, Kpb(u) + K("MASK4"), K("AB", u, [0, 1]))
                for u in range(8):
                    if ABSPLIT:
                        tt(POOL, AB[u][:, 256:512], AB[u][:, 256:512], MASK4[:, 256:512], ALU.mult,
                           K("AB", u, 1) + K("MASK4"), K("AB", u, 1))
                for u in range(8):
                    tr(pb[u][:].bitcast(BF16)[:, 0:128], AB[u][:, 0:128], IDB[:], K("AB", u, 0) + K("IDB"), Kpb(u))
                for u in range(8):
                    cp(ACT, NN[u][:], pb[u][:].bitcast(BF16)[:, 0:128], Kpb(u), K("NN", u))
                    tt(DVE, PT[u], AB[u][:, 0:128], IDB[:], ALU.add, K("AB", u, 0) + K("IDB"), K("PTP", u // 2))
                for b in range(1, 8):
                    for p_ in range(4):
                        for i in range(2):
                            u = 2 * p_ + i
                            o = 256 * i
                            if b == 1:
                                Mn, Mt, kin = NN[u][:], AB[u][:, 0:128], K("NN", u) + K("AB", u, 0)
                            else:
                                Mn, Mt, kin = MMP[p_][:, o:o + 128], MMP[p_][:, o + 128:o + 256], K("MMP", p_)
                            if b <= 6:
                                mm(pb[p_][:, o:o + 128], Mt, Mn, True, True, kin, Kpb(p_))
                            if b <= 5:
                                mm(pb[p_][:, o + 128:o + 256], Mn, Mt, True, True, kin, Kpb(p_))
                            if b >= 2:
                                mm(pb[4 + p_][:, i * 128:(i + 1) * 128], Mn, PT[u], True, True, kin + K("PTP", p_), Kpb(4 + p_))
                    for p_ in range(4):
                        ev_ = DVE if p_ == 3 else ACT
                        if b <= 5:
                            cp(ev_, MMP[p_][:], pb[p_][:], Kpb(p_), K("MMP", p_))
                        elif b == 6:
                            cp(ev_, MMP[p_][:].rearrange("p (i x) -> p i x", i=2)[:, :, 0:128],
                               pb[p_][:].rearrange("p (i x) -> p i x", i=2)[:, :, 0:128], Kpb(p_), K("MMP", p_))
                        if b >= 2:
                            tt(DVE, PTP[p_][:], pb[4 + p_][:, 0:256], PTP[p_][:], ALU.add, Kpb(4 + p_) + K("PTP", p_), K("PTP", p_))
                cp(ACT, STB[0][:], S32[l][:, j, :], K("S32", l, j), K("STB", 0))
                for c in range(4):
                    sbi = c % 2
                    act(STG[:], S32[l][:, j, :], AF.Copy, K("S32", l, j) + K("GC"), K("STG"), scale=GC[:, c:c + 1])
                    for h in range(2):
                        u, hs = c * 2 + h, slice(64 * h, 64 * h + 64)
                        xo = pb[4][:, h * 64:(h + 1) * 64]
                        mm(xo, AB[u][:, 256:384], VTOK[:, c, hs], True, False, K("AB", u, 1) + K("VTOK"), Kpb(4))
                        mm(xo, ART[hs, c, 0, :], STB[sbi][hs, :], False, True, K("ART", 0) + K("STB", sbi), Kpb(4))
                    emit_fill(3)
                    cp(ACT, XSB[:].rearrange("p h v -> p (h v)"), pb[4][:, 0:128], Kpb(4), K("XSB"))
                    for h in range(2):
                        u = c * 2 + h
                        mm(pb[5][:, h * 64:(h + 1) * 64], PT[u], XSB[:, h, :], True, True, K("PTP", u // 2) + K("XSB"), Kpb(5))
                    emit_fill(3)
                    cp(DVE, USB[:].rearrange("p h v -> p (h v)"), pb[5][:, 0:128], Kpb(5), K("USB"))
                    for h in range(2):
                        u, hs = c * 2 + h, slice(64 * h, 64 * h + 64)
                        so = pb[6][hs, 0:64]
                        mm(so, BKTOK[:, c, 0, hs], USB[:, h, :], True, False, K("BKTOK") + K("USB"), Kpb(6))
                        mm(so, BKTOK[:, c, 1, hs], VTOK[:, c, hs], False, True, K("BKTOK") + K("VTOK"), Kpb(6))
                    for h in range(2):
                        u, hs = c * 2 + h, slice(64 * h, 64 * h + 64)
                        yo = pb[7][:, u * 64:(u + 1) * 64]
                        mm(yo, ART[hs, c, 1, :], STB[sbi][hs, :], True, False, K("ART", 1) + K("STB", sbi), Kpb(7))
                        mm(yo, AB[u][:, 128:256], USB[:, h, :], False, False, K("AB", u, 0) + K("USB"), Kpb(7))
                        mm(yo, AB[u][:, 384:512], VTOK[:, c, hs], False, True, K("AB", u, 1) + K("VTOK"), Kpb(7))
                    emit_fill(2)
                    stt(S32[l][:, j, :], pb[6][:, 0:64], GC[:, c:c + 1], STG[:], ALU.mult, ALU.add,
                        Kpb(6) + K("GC") + K("STG"), K("S32", l, j))
                    cp(ACT, STB[1 - sbi][:], S32[l][:, j, :], K("S32", l, j), K("STB", 1 - sbi))
                emit_fill(len(fill))
                if nxt_key is not None:
                    bstate["pre"] = nxt_key
                if nxt_key is not None and ESHIFT:
                    for q in range(4):
                        shift(l, q * 4 + nxt_key[1], q, q, G[q], KG(q))
                    bstate["shifted"] = nxt_key
                P.begin_capture()
                y3 = pb[7][:].rearrange("p (g v) -> p g v", g=8)
                P.add(DVE, lambda e: e.tensor_reduce(out=SUM8[:], in_=y3, axis=AX.X, op=ALU.add), Kpb(7), K("SUM8"))
                for u in range(8):
                    act(JNK[:], pb[7][:, u * 64:(u + 1) * 64], AF.Square, Kpb(7), K("JNK") + K("SSQ8"),
                        accum_out=SSQ8[:, u:u + 1])
                ts(DVE, M8[:], SUM8[:], 1.0 / 64, None, ALU.mult, None, K("SUM8"), K("M8"))
                tt(DVE, V8[:], M8[:], M8[:], ALU.mult, K("M8"), K("V8"))
                stt(V8[:], SSQ8[:], 1.0 / 64, V8[:], ALU.mult, ALU.subtract, K("SSQ8") + K("V8"), K("V8"))
                act(V8[:], V8[:], AF.Ln, K("V8"), K("V8"), bias=GN_EPS)
                act(V8[:], V8[:], AF.Exp, K("V8"), K("V8"), scale=-0.5)
                for u, (c, h) in enumerate(units):
                    ts(DVE, YN[:, c, h * 64:(h + 1) * 64], pb[7][:, u * 64:(u + 1) * 64], M8[:, u:u + 1], V8[:, u:u + 1],
                       ALU.subtract, ALU.mult, Kpb(7) + K("M8") + K("V8"), K("YN"))
                for c in range(4):
                    tr(pb[3][:, c * 128:(c + 1) * 128], YN[:, c, :], IDF[:], K("YN") + K("IDF"), Kpb(3))
                YNf = YN[:].rearrange("p c v -> p (c v)")
                stt(YNf, pb[3][:], col(l, 38 + j), BONp, ALU.mult, ALU.add, Kpb(3) + K("COLS") + kbon, K("YN"))
                stt(YA[:, j, nt], YNf, col(l, 42 + j), ZGp, ALU.add, ALU.mult, K("YN") + kzg + K("COLS"), K("YA", j, n))
                bstate["gn"] = P.end_capture()
                if ws_next is None and n == 1:
                    for it_ in bstate["gn"]:
                        P.replay(it_)
                    bstate["gn"] = None

        def phase_C(l, j, ws):
            for n in range(2):
                nt = slice(n * 512, (n + 1) * 512)
                bb, gb = 4 * n, 4 * n
                for q in range(4):
                    inproj(bb + q, ws[q], n)
                U32, Y, SZ = G[gb], G[gb + 1], G[gb + 2]
                cp(ACT, U32, pb[bb + 2][:], Kpb(bb + 2), KG(gb))
                cp(ACT, BQ[n][:, 0:2], CCAR[l][:, j, :], K("CCAR", l, j), K("BQ", n))
                tt(DVE, BQ[n][:, 2:514], pb[bb + 1][:], U32, ALU.mult, Kpb(bb + 1) + KG(gb), K("BQ", n))
                cp(ACT, CCAR[l][:, j, :], BQ[n][:, 512:514], K("BQ", n), K("CCAR", l, j))
                ts(DVE, Y, BQ[n][:, 0:512], col(l, 50 + j), None, ALU.mult, None, K("BQ", n) + K("COLS"), KG(gb + 1))
                stt(Y, BQ[n][:, 1:513], col(l, 54 + j), Y, ALU.mult, ALU.add, K("BQ", n) + K("COLS") + KG(gb + 1), KG(gb + 1))
                stt(Y, BQ[n][:, 2:514], col(l, 58 + j), Y, ALU.mult, ALU.add, K("BQ", n) + K("COLS") + KG(gb + 1), KG(gb + 1))
                act(SZ, pb[bb + 3][:], AF.Silu, Kpb(bb + 3), KG(gb + 2))
                tt(DVE, Y, pb[bb][:], Y, ALU.mult, Kpb(bb) + KG(gb + 1), KG(gb + 1))
                tt(DVE, YB[:, j, nt], Y, SZ, ALU.mult, KG(gb + 1, gb + 2), K("YB", j, n))

        def phase_D(l, g, ws, first_half):
            W = POOL_WINDOWS[g]
            for n in range(2):
                nt = slice(n * 512, (n + 1) * 512)
                bb, gb = 4 * n, 4 * n
                inproj(bb, ws[0], n)
                inproj(bb + 1, ws[1], n)
                cp(ACT, BQ[n][:, 0:15], PCAR[l][:, g, :], K("PCAR", l, g), K("BQ", n))
                cp(ACT, BQ[n][:, 15:527], pb[bb][:], Kpb(bb), K("BQ", n))
                cp(ACT, PCAR[l][:, g, :], BQ[n][:, 512:527], K("BQ", n), K("PCAR", l, g))
                cur, curk, lo = BQ[n], K("BQ", n), 0
                for k in (1, 2, 4, 8):
                    if k >= W:
                        break
                    ni = 2 if curk != K("BQ", 2) else 3
                    nxt, nxtk = BQ[ni], K("BQ", ni)
                    tt(DVE, nxt[:, lo + k:527], cur[:, lo + k:527], cur[:, lo:527 - k], ALU.add, curk, nxtk)
                    cur, curk, lo = nxt, nxtk, lo + k
                stt(PLB[n][:], cur[:, 15:527], 1.0 / W, BQ[n][:, 15:527], ALU.mult, ALU.subtract, curk + K("BQ", n), K("PLB", n))
                if first_half and n == 0:
                    TT = G[gb + 2]
                    tt(DVE, TT[:, 0:16], cur[:, 15:31], ICNT[:, g, :], ALU.mult, curk + K("ICNT"), KG(gb + 2))
                    tt(DVE, PLB[n][:, 0:16], TT[:, 0:16], BQ[n][:, 15:31], ALU.subtract, KG(gb + 2) + K("BQ", n), K("PLB", n))
                mm(pb[bb + 2][:], PW[l][:, g, :], PLB[n][:], True, True, K("PW", l) + K("PLB", n), Kpb(bb + 2))
                SZ = G[gb + 1]
                act(SZ, pb[bb + 1][:], AF.Silu, Kpb(bb + 1), KG(gb + 1))
                stt(YC[:, g, nt], pb[bb + 2][:], col(l, 62 + g), SZ, ALU.mult, ALU.mult,
                    Kpb(bb + 2) + K("COLS") + KG(gb + 1), K("YC", g, n))

        ecnt = {"c": 0}

        def phase_E(l, dc, wg, wbr):
            Ys = [(YA, "YA"), (YB, "YB"), (YC, "YC")]
            for n in range(2):
                nt = slice(n * 512, (n + 1) * 512)
                ai = 2 + (ecnt["c"] // 3) % 2
                ACC, kacc = BQ[ai][:, 0:512], K("BQ", ai)
                for nb in range(3):
                    bp = (ecnt["c"] % 4) * 2
                    si = ecnt["c"] % 2
                    ecnt["c"] += 1
                    S, ks = BQ[si][:, 0:512], K("BQ", si)
                    inproj(bp, wg[nb], n)
                    wt_, k0, wk_ = wbr[nb]
                    Yt, yk = Ys[nb]
                    for kc in range(4):
                        mm(pb[bp + 1][:], wt_[:, k0 + kc, :], Yt[:, kc, nt], kc == 0, kc == 3,
                           wk_ + K(yk, kc, n), Kpb(bp + 1))
                    act(S, pb[bp][:], AF.Sigmoid, Kpb(bp), ks)
                    if nb == 0:
                        tt(DVE, ACC, pb[bp + 1][:], S, ALU.mult, Kpb(bp + 1) + ks, kacc)
                    else:
                        tt(DVE, S, pb[bp + 1][:], S, ALU.mult, Kpb(bp + 1) + ks, ks)
                        if nb == 1:
                            tt(DVE, ACC, ACC, S, ALU.add, kacc + ks, kacc)
                        else:
                            tt(EADD, MG[:, dc, nt], ACC, S, ALU.add, kacc + ks, KG(dc))

        def phase_F(l):
            for t in range(8):
                bp = (t % 4) * 2
                for hf in range(2):
                    for kc in range(8):
                        mm(pb[bp + hf][:], MG[:, kc, t * 128:(t + 1) * 128], WO[:, kc, hf * 512:(hf + 1) * 512], kc == 0, kc == 7,
                           KG(*range(16)), Kpb(bp + hf))
                jk = t % 2
                for hf in range(2):
                    act(BQ[jk][:, 0:512], pb[bp + hf][:], AF.Square, Kpb(bp + hf), K("BQ", jk) + K("SS2", hf),
                        accum_out=SS2[:, hf:hf + 1])
                tt(DVE, SS2[:, 0:1], SS2[:, 0:1], SS2[:, 1:2], ALU.add, K("SS2", [0, 1]), K("SS2", 0))
                act(RS2[:], SS2[:, 0:1], AF.Ln, K("SS2", 0), K("RS2"), scale=1.0 / D_MODEL, bias=NORM_EPS)
                act(RS2[:], RS2[:], AF.Exp, K("RS2"), K("RS2"), scale=-0.5)
                for hf in range(2):
                    T = BQ[2 + hf][:, 0:512]
                    hs = slice(hf * 512, (hf + 1) * 512)
                    tt(DVE, T, pb[bp + hf][:], GPOST[:, hs], ALU.mult, Kpb(bp + hf) + K("GPOST"), K("BQ", 2 + hf))
                    stt(XS[:, t, hs], T, RS2[:], XS[:, t, hs], ALU.mult, ALU.add, K("BQ", 2 + hf) + K("RS2") + K("XS", t), K("XS", t))

        stages = []

        stage_names = []

        def add_stage(load, compute, name="?"):
            stages.append((load, compute))
            stage_names.append(name)

        for s in range(n_seq):
            for hf in range(n_half):
                first_half = hf == 0
                base = s * SEQ + hf * HALF

                def pass_begin(base=base, first_half=first_half):
                    P.dma(lambda e: e.dma_start(out=XS[:], in_=x_d[base:base + HALF, :].rearrange("(t p) d -> p t d", p=128)),
                          writes=K("XS", list(range(8))), dkey="xin")
                    if first_half:
                        for l in range(2):
                            P.add(POOL, lambda e, l=l: e.memset(S32[l][:], 0.0), (), K("S32", l, [0, 1, 2, 3]))
                            P.add(POOL, lambda e, l=l: e.memset(CAR[l][:], 0.0), (), K("CAR", l, list(range(18))))
                            P.add(POOL, lambda e, l=l: e.memset(CCAR[l][:], 0.0), (), K("CCAR", l, [0, 1, 2, 3]))
                            P.add(POOL, lambda e, l=l: e.memset(PCAR[l][:], 0.0), (), K("PCAR", l, [0, 1, 2, 3]))

                for l in range(n_layers):
                    h = {}

                    def ld0(l=l, h=h):
                        ws = load_in_chunks(l, [2048])
                        if l == 1:
                            s_ = wload(vdn_d.rearrange("(k p) n -> p k n", p=128), ncol=32)
                            ws.append(wcast(s_, ncol=32))
                        h["b0"] = ws

                    def c0(l=l, h=h, is_first=(l == 0), pb_=pass_begin):
                        if is_first:
                            pb_()
                        P.dma(lambda e: e.dma_start(out=GPRE[:], in_=gpre_d[l:l + 1, :].broadcast_to([128, D_MODEL])),
                              writes=K("GPRE"), dkey="gpre")
                        P.dma(lambda e: e.dma_start(out=GPOST[:], in_=gpost_d[l:l + 1, :].broadcast_to([128, D_MODEL])),
                              writes=K("GPOST"), dkey="gpost")
                        phase_A(l)
                        phase_B0(l, h["b0"])

                    add_stage(ld0, c0, "A")
                    for j in range(4):
                        def ldB(l=l, j=j, h=h):
                            h["B", j] = load_in_chunks(l, [q * 512 + j * 128 for q in range(4)])
                        add_stage(ldB, lambda l=l, j=j, h=h, fh=first_half: phase_B(l, j, h["B", j], fh, h.get(("B", j + 1))), "B")
                    def ld_wo(l, ec):
                        wov = w_out_d[l].rearrange("(k p) e -> p k e", p=128)
                        s_ = wload(wov[:, :, ec * 128:(ec + 1) * 128])
                        wcast(s_, dst=WO[:, :, ec * 128:(ec + 1) * 128], dst_keys=KG(*range(8, 16)))

                    for j in range(4):
                        def ldC(l=l, j=j, h=h):
                            h["C", j] = load_in_chunks(l, [2176 + q * 512 + j * 128 for q in range(4)])
                            if j >= 1:
                                ld_wo(l, j - 1)
                        add_stage(ldC, lambda l=l, j=j, h=h: phase_C(l, j, h["C", j]), "C")
                    for g in range(4):
                        def ldD(l=l, g=g, h=h):
                            h["D", g] = load_in_chunks(l, [4224 + q * 512 + g * 128 for q in range(2)])
                            ld_wo(l, 3 + g)
                        add_stage(ldD, lambda l=l, g=g, h=h, fh=first_half: phase_D(l, g, h["D", g], fh), "D")
                    for dc in range(8):
                        def ldE(l=l, dc=dc, h=h):
                            wg = load_in_chunks(l, [5248 + nb * 1024 + dc * 128 for nb in range(3)])
                            brv = w_br_d[l].rearrange("n (k p) d -> n p k d", p=128)
                            s01 = wload(brv[0][:, :, dc * 128:(dc + 1) * 128], nk=4, k0=0)
                            wload(brv[1][:, :, dc * 128:(dc + 1) * 128], nk=4, k0=4, same_stage=True)
                            w01 = wcast(s01)
                            s2 = wload(brv[2][:, :, dc * 128:(dc + 1) * 128], nk=4, k0=0)
                            wcast(s2, nk=4, dst=WBR2[dc % 2][:], dst_keys=K("wbr2", dc % 2))
                            if dc == 0:
                                ld_wo(l, 7)
                            h["E", dc] = (wg, [(WBF[w01], 0, K("wb", w01)), (WBF[w01], 4, K("wb", w01)),
                                               (WBR2[dc % 2], 0, K("wbr2", dc % 2))])
                        add_stage(ldE, lambda l=l, dc=dc, h=h: phase_E(l, dc, *h["E", dc]), "E")

                    def ldF(l=l):
                        pass

                    def cF(l=l, last=(l == n_layers - 1), base=base):
                        phase_F(l)
                        if last:
                            P.dma(lambda e: e.dma_start(out=y_d[base:base + HALF, :].rearrange("(t p) d -> p t d", p=128), in_=XS[:]),
                                  reads=K("XS", list(range(8))), dkey="xout")

                    add_stage(ldF, cF, "F")

        if dbg:
            pass
        global _LASTP
        _LASTP = P
        del _MARKS[:]
        stages[0][0]()
        for i, (ld, comp) in enumerate(stages):
            if i + 1 < len(stages):
                stages[i + 1][0]()
            _MARKS.append((stage_names[i], len(P.ops)))
            comp()
            if dbg and i == 12:
                for bi, (Yt, yk) in enumerate([(YA, "YA"), (YB, "YB"), (YC, "YC")]):
                    for jj in range(4):
                        for n in range(2):
                            cp(DVE, G[0], Yt[:, jj, n * 512:(n + 1) * 512], K(yk, jj, n), KG(0))
                            P.dma(lambda e, bi=bi, jj=jj, n=n: e.dma_start(out=dbg_d[bi, :, jj, n * 512:(n + 1) * 512], in_=G[0]),
                                  reads=KG(0), dkey="dbg")
        stats = P.emit(nc, sems, dsem)
    return nc, stats


def _colpack(v):
    v = np.asarray(v, np.float32).reshape(-1)
    n = (v.size + 127) // 128
    out = np.zeros((n * 128,), np.float32)
    out[:v.size] = v
    return out.reshape(n, 128).T


def host_consts():
    s = np.arange(128)[:, None]
    t = np.arange(128)[None, :]
    strict = (s < t).astype(np.float32)
    incl = (s <= t).astype(np.float32)
    mask4 = np.concatenate([strict, incl, strict, incl], axis=1)
    maskl = (t < s).astype(np.float32)
    bones = np.zeros((128, 128), np.float32)
    bones[:64, :64] = 1.0
    bones[64:, 64:] = 1.0
    rmask = np.ones((128, 512), np.float32)
    rmask[:, ::128] = 0.0
    icnt = np.zeros((128, 4, 16), np.float32)
    for g, w in enumerate(POOL_WINDOWS):
        icnt[:, g, :] = 1.0 / np.minimum(np.arange(1, 17), w)
    return dict(identf=np.eye(128, dtype=np.float32), mask4=mask4, maskl=maskl, bones=bones, rmask=rmask,
                icnt=icnt.reshape(128, 64))


def host_pack(inp):
    f = lambda a: np.ascontiguousarray(np.asarray(a, np.float32))
    cols = []
    for l in range(2):
        cl = [_colpack(inp["mu_shift"][l])]
        cl.append(_colpack(inp["vres_mu"][0]) if l == 1 else np.zeros((128, 1), np.float32))
        for nm in ["rwkv_w0", "rwkv_a0", "rwkv_k_k", "rwkv_k_a", "rwkv_r_k", "rwkv_gn_w", "rwkv_gn_b"]:
            cl.append(_colpack(inp[nm][l]))
        cl.append(_colpack(inp["vres_v0"][0]) if l == 1 else np.zeros((128, 4), np.float32))
        for i in range(3):
            cl.append(_colpack(inp["conv_w"][l][i]))
        cl.append(_colpack(inp["pool_scale"][l]))
        c = np.concatenate(cl, axis=1)
        assert c.shape == (128, NCL), c.shape
        cols.append(c)
    shared = dict(
        w_in=f(inp["w_in"]), w_branch=f(inp["w_branch"]), w_out=f(inp["w_out"]),
        vres_down=f(inp["vres_down"][0]),
        lw=f(np.concatenate([inp["rwkv_w_up"], inp["rwkv_a_up"]], axis=1)),
        vres_up=f(inp["vres_up"][0]), pool_w=f(inp["pool_w"]),
        cols=f(np.concatenate(cols, axis=1)),
        gpre=f(inp["pre_norm_w"]), gpost=f(inp["post_norm_w"]),
    )
    shared.update(host_consts())
    return shared


_CACHE = {}
_MARKS = []
_LASTP = None


def kernel(**inputs):
    x = np.asarray(inputs["x"], np.float32)
    B = x.shape[0]
    ncore = 8
    per = B // ncore
    shared = host_pack(inputs)
    if "nc" not in _CACHE:
        _CACHE["nc"] = build_program(n_seq=per)[0]
    nc = _CACHE["nc"]
    in_maps = []
    for i in range(ncore):
        m = dict(shared)
        m["x"] = np.ascontiguousarray(x[i * per:(i + 1) * per].reshape(per * SEQ, D_MODEL))
        in_maps.append(m)
    res = run_bass_kernel_spmd(nc, in_maps, core_ids=list(range(ncore)))
    out = np.concatenate([r["y"].reshape(per, SEQ, D_MODEL) for r in res.results], axis=0)
    return out.astype(np.float32)
```
